# Optimizing a Trainium2 kernel written in Bass

```python
import jax, jax.numpy as jnp
from jax import lax
import numpy as np

D_MODEL = 1024
BATCH = 8
SEQ = 2048
DEPTH = 1

CHUNK = 64
D_POOL = D_MODEL // 2
POOL_WINDOWS = (2, 4, 8, 16)
POOL_GROUPS = len(POOL_WINDOWS)
POOL_GC = D_POOL // POOL_GROUPS
D_MLSTM = D_MODEL // 2
N_HEADS = 4
HEAD_DIM = D_MLSTM // N_HEADS
CONV_K = 4
N_GROUPS = 4
EXPERTS_PER_GROUP = 8
N_EXPERTS = N_GROUPS * EXPERTS_PER_GROUP
TOP_K = 2
D_EXPERT = D_MODEL // 2
MOE_BLOCK = 128
EPS = 1e-6
SPLIT_SIZES = (D_POOL, D_MLSTM, D_MLSTM, D_MLSTM, D_MLSTM, N_HEADS, N_HEADS, D_MODEL, D_MODEL)
N_IN = sum(SPLIT_SIZES)

kernel_name = "hybrid_pool_mlstm_hmoe_block"


def rmsnorm(x, g):
    xf = x.astype(jnp.float32)
    y = xf * lax.rsqrt(jnp.mean(xf * xf, axis=-1, keepdims=True) + EPS) * g.astype(jnp.float32)
    return y.astype(x.dtype)


def pool_mixer(u, w_pool, pool_scale):
    B, S, _ = u.shape
    uf = u.astype(jnp.float32)
    cs = jnp.pad(jnp.cumsum(uf, axis=1), ((0, 0), (1, 0), (0, 0)))
    t = jnp.arange(S)
    outs = []
    for g, w in enumerate(POOL_WINDOWS):
        sl = slice(g * POOL_GC, (g + 1) * POOL_GC)
        start = jnp.maximum(t + 1 - w, 0)
        win_sum = cs[:, 1:, sl] - cs[:, start, sl]
        cnt = (t + 1 - start).astype(jnp.float32)
        outs.append(win_sum / cnt[None, :, None] - uf[:, :, sl])
    d = jnp.stack(outs, axis=2)
    y = jnp.einsum('bsgc,gcd->bsgd', d, w_pool.astype(jnp.float32)).reshape(B, S, D_POOL)
    return (y * pool_scale.astype(jnp.float32)).astype(u.dtype)


def causal_conv(u, w):
    S = u.shape[1]
    up = jnp.pad(u, ((0, 0), (CONV_K - 1, 0), (0, 0)))
    return sum(up[:, j:j + S, :] * w[j] for j in range(CONV_K))


def mlstm_chunkwise(q, k, v, i_pre, f_pre):
    B, S, H, Dh = q.shape
    NC, L = S // CHUNK, CHUNK

    def to_chunks(a):
        a = a.astype(jnp.float32).reshape((B, NC, L, H) + a.shape[3:])
        return jnp.moveaxis(a, 3, 1)

    q = to_chunks(q)
    k = to_chunks(k) * (Dh ** -0.5)
    v = to_chunks(v)
    ig = to_chunks(i_pre)
    lf = jax.nn.log_sigmoid(to_chunks(f_pre))
    b = jnp.cumsum(lf, axis=-1)
    b_last = b[..., -1]

    a_log = b_last[..., None] - b + ig
    a_max = jnp.max(a_log, axis=-1)
    w_loc = jnp.exp(a_log - a_max[..., None])
    C_loc = jnp.einsum('bhcld,bhcle->bhcde', w_loc[..., None] * k, v)
    n_loc = jnp.einsum('bhcl,bhcld->bhcd', w_loc, k)

    def step(carry, inp):
        C, n, m = carry
        bl, am, Cl, nl = inp
        m_new = jnp.maximum(bl + m, am)
        s_prev = jnp.exp(bl + m - m_new)
        s_loc = jnp.exp(am - m_new)
        C_new = s_prev[..., None, None] * C + s_loc[..., None, None] * Cl
        n_new = s_prev[..., None] * n + s_loc[..., None] * nl
        return (C_new, n_new, m_new), (C, n, m)

    init = (jnp.zeros((B, H, Dh, Dh), jnp.float32), jnp.zeros((B, H, Dh), jnp.float32),
            jnp.zeros((B, H), jnp.float32))
    xs = (jnp.moveaxis(b_last, 2, 0), jnp.moveaxis(a_max, 2, 0),
          jnp.moveaxis(C_loc, 2, 0), jnp.moveaxis(n_loc, 2, 0))
    _, (C_prev, n_prev, m_prev) = lax.scan(step, init, xs)
    C_prev = jnp.moveaxis(C_prev, 0, 2)
    n_prev = jnp.moveaxis(n_prev, 0, 2)
    m_prev = jnp.moveaxis(m_prev, 0, 2)

    inter_log = b + m_prev[..., None]
    causal = jnp.tril(jnp.ones((L, L), dtype=bool))
    D = b[..., :, None] - b[..., None, :] + ig[..., None, :]
    D = jnp.where(causal, D, -jnp.inf)
    m_t = jnp.maximum(inter_log, jnp.max(D, axis=-1))
    w_inter = jnp.exp(inter_log - m_t)
    s = jnp.einsum('bhctd,bhcsd->bhcts', q, k) * jnp.exp(D - m_t[..., None])
    num = (w_inter[..., None] * jnp.einsum('bhctd,bhcde->bhcte', q, C_prev)
           + jnp.einsum('bhcts,bhcse->bhcte', s, v))
    nq = w_inter * jnp.einsum('bhctd,bhcd->bhct', q, n_prev) + jnp.sum(s, axis=-1)
    den = jnp.maximum(jnp.abs(nq), jnp.exp(-m_t))
    h = num / den[..., None]
    return jnp.moveaxis(h, 1, 3).reshape(B, S, H, Dh)


def head_layernorm(h, g):
    B, S, H, Dh = h.shape
    mu = jnp.mean(h, axis=-1, keepdims=True)
    var = jnp.mean(jnp.square(h - mu), axis=-1, keepdims=True)
    hn = (h - mu) * lax.rsqrt(var + EPS)
    return hn * g.astype(jnp.float32).reshape(H, Dh)


def hybrid_mixer(h, w_in, b_if, conv_q, conv_k, g_head, w_pool, pool_scale, w_br_a, w_br_b, w_out):
    B, S, _ = h.shape
    z = h @ w_in
    idx = [int(c) for c in np.cumsum(SPLIT_SIZES)[:-1]]
    u_pool, q, k, v, o, i_pre, f_pre, ga, gb = jnp.split(z, idx, axis=-1)
    y_a = pool_mixer(u_pool, w_pool, pool_scale) @ w_br_a
    q = jax.nn.silu(causal_conv(q, conv_q)).reshape(B, S, N_HEADS, HEAD_DIM)
    k = jax.nn.silu(causal_conv(k, conv_k)).reshape(B, S, N_HEADS, HEAD_DIM)
    v = v.reshape(B, S, N_HEADS, HEAD_DIM)
    i_pre = i_pre.astype(jnp.float32) + b_if[:N_HEADS].astype(jnp.float32)
    f_pre = f_pre.astype(jnp.float32) + b_if[N_HEADS:].astype(jnp.float32)
    hm = mlstm_chunkwise(q, k, v, i_pre, f_pre)
    hm = head_layernorm(hm, g_head).reshape(B, S, D_MLSTM)
    hm = (jax.nn.sigmoid(o.astype(jnp.float32)) * hm).astype(h.dtype)
    y_b = hm @ w_br_b
    merged = jax.nn.sigmoid(ga) * y_a + jax.nn.sigmoid(gb) * y_b
    return merged @ w_out


def hier_moe(h, w_rg, b_rg, w_re, b_re, w_gate, w_up, w_down):
    B, S, D = h.shape
    T = B * S
    xt = h.reshape(T, D)
    g_logits = (xt @ w_rg).astype(jnp.float32) + b_rg.astype(jnp.float32)
    g_prob = jax.nn.softmax(g_logits, axis=-1)
    g_sel = jnp.argmax(g_logits, axis=-1)
    p_g = jnp.take_along_axis(g_prob, g_sel[:, None], axis=-1)
    e_logits = ((xt @ w_re).astype(jnp.float32) + b_re.astype(jnp.float32)).reshape(
        T, N_GROUPS, EXPERTS_PER_GROUP)
    e_sel = jnp.take_along_axis(e_logits, g_sel[:, None, None], axis=1)[:, 0]
    top_p, top_i = lax.top_k(jax.nn.softmax(e_sel, axis=-1), TOP_K)
    gate = p_g * top_p / jnp.sum(top_p, axis=-1, keepdims=True)
    expert_id = g_sel[:, None] * EXPERTS_PER_GROUP + top_i

    A = T * TOP_K
    flat_e = expert_id.reshape(A)
    flat_tok = jnp.repeat(jnp.arange(T), TOP_K)
    flat_w = gate.reshape(A)
    order = jnp.argsort(flat_e)
    e_s, tok_s, w_s = flat_e[order], flat_tok[order], flat_w[order]
    counts = jnp.bincount(flat_e, length=N_EXPERTS)
    padded = (counts + MOE_BLOCK - 1) // MOE_BLOCK * MOE_BLOCK
    off = jnp.cumsum(counts) - counts
    pend = jnp.cumsum(padded)
    poff = pend - padded
    dest = poff[e_s] + jnp.arange(A) - off[e_s]
    n_rows = A + N_EXPERTS * MOE_BLOCK
    n_blocks = n_rows // MOE_BLOCK
    buf = jnp.zeros((n_rows, D), h.dtype).at[dest].set(xt[tok_s])
    block_e = jnp.clip(jnp.searchsorted(pend, jnp.arange(n_blocks) * MOE_BLOCK, side='right'),
                       0, N_EXPERTS - 1)

    def run_block(args):
        xb, e = args
        hid = jax.nn.silu(xb @ w_gate[e]) * (xb @ w_up[e])
        return hid @ w_down[e]

    yb = lax.map(run_block, (buf.reshape(n_blocks, MOE_BLOCK, D), block_e)).reshape(n_rows, D)
    y = jnp.zeros((T, D), jnp.float32).at[tok_s].add(yb[dest].astype(jnp.float32) * w_s[:, None])
    return y.reshape(B, S, D).astype(h.dtype)


def setup_inputs(seed: int = 0) -> dict:
    key = jax.random.key(seed)
    ks = jax.random.split(key, 24)
    nrm = jax.random.normal
    f32 = jnp.float32
    x = nrm(ks[0], (BATCH, SEQ, D_MODEL), f32)
    g_mix = 1.0 + 0.1 * nrm(ks[1], (DEPTH, D_MODEL), f32)
    w_in = nrm(ks[2], (DEPTH, D_MODEL, N_IN), f32) * D_MODEL ** -0.5
    b_i = 0.1 * nrm(ks[3], (DEPTH, N_HEADS), f32)
    b_f = jnp.linspace(3.0, 6.0, N_HEADS, dtype=f32)[None] + 0.1 * nrm(ks[4], (DEPTH, N_HEADS), f32)
    b_if = jnp.concatenate([b_i, b_f], axis=-1)
    conv_q = nrm(ks[5], (DEPTH, CONV_K, D_MLSTM), f32) * CONV_K ** -0.5
    conv_k = nrm(ks[6], (DEPTH, CONV_K, D_MLSTM), f32) * CONV_K ** -0.5
    g_head = 1.0 + 0.1 * nrm(ks[7], (DEPTH, D_MLSTM), f32)
    w_pool = nrm(ks[8], (DEPTH, POOL_GROUPS, POOL_GC, POOL_GC), f32) * POOL_GC ** -0.5
    pool_scale = 1.0 + 0.1 * nrm(ks[9], (DEPTH, D_POOL), f32)
    w_br_a = nrm(ks[10], (DEPTH, D_POOL, D_MODEL), f32) * D_POOL ** -0.5
    w_br_b = nrm(ks[11], (DEPTH, D_MLSTM, D_MODEL), f32) * D_MLSTM ** -0.5
    w_out = nrm(ks[12], (DEPTH, D_MODEL, D_MODEL), f32) * D_MODEL ** -0.5
    g_ffn = 1.0 + 0.1 * nrm(ks[13], (DEPTH, D_MODEL), f32)
    w_rg = nrm(ks[14], (DEPTH, D_MODEL, N_GROUPS), f32) * D_MODEL ** -0.5
    b_rg = 0.01 * nrm(ks[15], (DEPTH, N_GROUPS), f32)
    w_re = nrm(ks[16], (DEPTH, D_MODEL, N_EXPERTS), f32) * D_MODEL ** -0.5
    b_re = 0.01 * nrm(ks[17], (DEPTH, N_EXPERTS), f32)
    w_e_gate = nrm(ks[18], (DEPTH, N_EXPERTS, D_MODEL, D_EXPERT), f32) * D_MODEL ** -0.5
    w_e_up = nrm(ks[19], (DEPTH, N_EXPERTS, D_MODEL, D_EXPERT), f32) * D_MODEL ** -0.5
    w_e_down = nrm(ks[20], (DEPTH, N_EXPERTS, D_EXPERT, D_MODEL), f32) * D_EXPERT ** -0.5
    g_final = 1.0 + 0.1 * nrm(ks[21], (D_MODEL,), f32)
    return {"x": x, "g_mix": g_mix, "w_in": w_in, "b_if": b_if, "conv_q": conv_q,
            "conv_k": conv_k, "g_head": g_head, "w_pool": w_pool, "pool_scale": pool_scale,
            "w_br_a": w_br_a, "w_br_b": w_br_b, "w_out": w_out, "g_ffn": g_ffn,
            "w_rg": w_rg, "b_rg": b_rg, "w_re": w_re, "b_re": b_re, "w_e_gate": w_e_gate,
            "w_e_up": w_e_up, "w_e_down": w_e_down, "g_final": g_final}


def reference(x, g_mix, w_in, b_if, conv_q, conv_k, g_head, w_pool, pool_scale, w_br_a, w_br_b,
              w_out, g_ffn, w_rg, b_rg, w_re, b_re, w_e_gate, w_e_up, w_e_down, g_final):
    for l in range(DEPTH):
        x = x + hybrid_mixer(rmsnorm(x, g_mix[l]), w_in[l], b_if[l], conv_q[l], conv_k[l],
                             g_head[l], w_pool[l], pool_scale[l], w_br_a[l], w_br_b[l], w_out[l])
        x = x + hier_moe(rmsnorm(x, g_ffn[l]), w_rg[l], b_rg[l], w_re[l], b_re[l],
                         w_e_gate[l], w_e_up[l], w_e_down[l])
    return rmsnorm(x, g_final)
```

```python
import math
import numpy as np
import ml_dtypes
import concourse.bass as bass
import concourse.mybir as mybir
from concourse.bass_utils import run_bass_kernel_spmd

F32 = mybir.dt.float32
BF16 = mybir.dt.bfloat16
I32 = mybir.dt.int32
AF = mybir.ActivationFunctionType
ALU = mybir.AluOpType
AX = mybir.AxisListType

D = 1024
S = 2048
NT = S // 128
NIN = 4616
NE = 32
DH = 128
EPS = 1e-6
POOL_WINDOWS = (2, 4, 8, 16)
STAGE = "full"
N_EXPERTS_RUN = NE

C_GMIX, C_GFFN, C_PSC, C_GHEAD, C_CQ, C_CK, C_BIF, C_BR, C_EPS, C_ONE = 0, 8, 16, 20, 24, 40, 56, 64, 100, 101
C_WR = 102
C_IDF = C_WR + 8 * 36
C_TRI = C_IDF + 128
C_ONES = C_TRI + 128
C_GFIN = C_ONES + 128
C_GFFN2 = C_GFIN + 1024
C_PIDX2 = C_GFFN2 + 8
C_THR16 = C_PIDX2 + 1
C_THR48 = C_THR16 + 16
NCPK = C_THR48 + 48
B_ID, B_PA, B_PB, B_PA0 = 0, 128, 640, 1152
B_TRIS, B_ONESB = 1664, 1792
NCBF = 1920
NBLK = 48
NROWS = NBLK * 256


class Tl:
    __slots__ = ("ap", "reg")

    def __init__(self, ap, reg):
        self.ap = ap
        self.reg = reg

    def __getitem__(self, idx):
        return Tl(self.ap[idx], self.reg)

    def v(self, ap):
        return Tl(ap, self.reg)

    def chunk(self, i, n):
        sp, s0, e0 = self.reg
        w = (e0 - s0) // n
        return Tl(self.ap[:, i], (sp, s0 + i * w, s0 + (i + 1) * w))


class Prog:
    LIMIT = 16000

    def __init__(self):
        self.ops = []
        self.recs = {}
        self._cap = None

    def capture(self, f):
        self._cap = []
        f()
        lst = self._cap
        self._cap = None
        return lst

    def merge(self, threads):
        threads = [t for t in threads if t]
        pos = [0] * len(threads)
        while True:
            best = None
            for k, t in enumerate(threads):
                if pos[k] < len(t):
                    fr = pos[k] / len(t)
                    if best is None or fr < best[0]:
                        best = (fr, k)
            if best is None:
                break
            k = best[1]
            self.add(*threads[k][pos[k]])
            pos[k] += 1

    def add(self, eng, fn, reads=(), writes=(), dma=False, grp="main"):
        if self._cap is not None:
            self._cap.append((eng, fn, tuple(reads), tuple(writes), dma, grp))
            return None
        idx = len(self.ops)
        deps = set()
        for t in reads:
            sp, s, e = t.reg
            for r in self.recs.get(sp, ()):
                if r[3] and r[0] < e and s < r[1]:
                    deps.add(r[2])
        for t in writes:
            sp, s, e = t.reg
            lst = self.recs.get(sp, [])
            keep = []
            for r in lst:
                if r[0] < e and s < r[1]:
                    deps.add(r[2])
                    if s <= r[0] and r[1] <= e:
                        continue
                keep.append(r)
            self.recs[sp] = keep
        tag = eng + ("_dma" if dma else "")
        for t in reads:
            sp, s, e = t.reg
            lst = self.recs.setdefault(sp, [])
            if not dma:
                for r in lst:
                    if (not r[3]) and r[0] == s and r[1] == e and r[4] == tag:
                        r[2] = idx
                        break
                else:
                    lst.append([s, e, idx, False, tag])
            else:
                lst.append([s, e, idx, False, tag])
        for t in writes:
            sp, s, e = t.reg
            self.recs.setdefault(sp, []).append([s, e, idx, True, tag])
        deps.discard(idx)
        self.ops.append(dict(eng=eng, fn=fn, deps=deps, dma=dma, grp=grp))
        return idx

    def emit(self, nc, block, eng_sems, dma_sems):
        ops = self.ops
        n = len(ops)
        dependents = [False] * n
        for i, o in enumerate(ops):
            for d in o["deps"]:
                od = ops[d]
                if od["eng"] == "pe" and o["eng"] == "pe" and not od["dma"] and not o["dma"]:
                    continue
                dependents[d] = True
        sig = [None] * n
        prev_dma = [None] * n
        cnt = {}
        dma_i = {g: 0 for g in dma_sems}
        uses = {g: [0] * len(v) for g, v in dma_sems.items()}
        for i, o in enumerate(ops):
            if o["fn"] is None:
                continue
            if o["dma"]:
                g = o["grp"]
                s = dma_i[g] % len(dma_sems[g])
                dma_i[g] += 1
                uses[g][s] += 1
                sig[i] = (dma_sems[g][s], 16 * uses[g][s])
                if uses[g][s] > 1:
                    prev_dma[i] = (dma_sems[g][s], 16 * (uses[g][s] - 1))
            elif dependents[i]:
                e = o["eng"]
                c = cnt.get(e, 0)
                cnt[e] = c + 1
                sig[i] = (eng_sems[e][c // self.LIMIT], c % self.LIMIT + 1)
        self.counts = cnt

        def make(engname):
            def body(eng):
                waited = {}
                for i, o in enumerate(ops):
                    if o["eng"] != engname:
                        continue
                    waits = []
                    for d in sorted(o["deps"]):
                        if sig[d] is None:
                            continue
                        od = ops[d]
                        if engname == "pe" and od["eng"] == "pe" and not od["dma"] and not o["dma"]:
                            continue
                        waits.append(sig[d])
                    if prev_dma[i] is not None:
                        waits.append(prev_dma[i])
                    for (sm, v) in waits:
                        if waited.get(sm.num, 0) < v:
                            eng.wait_ge(sm, v)
                            waited[sm.num] = v
                    if o["fn"] is not None:
                        ins = o["fn"](eng)
                        if sig[i] is not None:
                            ins.then_inc(sig[i][0], 16 if o["dma"] else 1)
            return body

        block.tensor(make("pe"))
        block.scalar(make("act"))
        block.vector(make("dve"))
        block.gpsimd(make("pool"))
        block.sync(make("sp"))


def build_nc():
    nc = bass.Bass("TRN2", target_bir_lowering=False)
    P = Prog()

    def din(name, shape, dt=F32):
        return nc.dram_tensor(name, list(shape), dt, kind="ExternalInput").ap()

    x_d = din("x", [S, D])
    win_d = din("w_in", [D, NIN])
    wpool_d = din("w_pool", [4, 128, 128])
    wbra_d = din("w_br_a", [512, D])
    wbrb_d = din("w_br_b", [512, D])
    wout_d = din("w_out", [D, D])
    weg_h = nc.dram_tensor("w_e_gate", [8192, 2048], F32, kind="ExternalInput")
    weu_h = nc.dram_tensor("w_e_up", [8192, 2048], F32, kind="ExternalInput")
    wed_h = nc.dram_tensor("w_e_down", [8192, 2048], F32, kind="ExternalInput")
    zx_d = din("zx", [128, 16384], BF16)
    wb_h = [nc.dram_tensor(f"wb{m}", [4096, 4096], BF16) for m in range(3)]
    xs_h = nc.dram_tensor("xs_s", [NROWS, D], BF16)
    ys_h = nc.dram_tensor("ys_s", [NROWS, D], F32)
    cpk_d = din("cpk", [128, NCPK])
    cbf_d = din("cbf", [128, NCBF], BF16)
    y_d = nc.dram_tensor("y", [S, D], F32, kind="ExternalOutput").ap()
    x1_d = nc.dram_tensor("x1s", [S, D], F32).ap()

    def dreg(name, ap, lo, hi):
        return Tl(ap, ("dram:" + name, lo, hi))

    ARENA_BYTES = 212480
    with (
        nc.sbuf_tensor("arena", [128, ARENA_BYTES // 4], F32) as arena,
        nc.psum_tensor("ps", [128, 8, 512], F32) as ps,
    ):
        def sview(off, shape, dt):
            esz = 4 if dt in (F32, I32) else 2
            nel = 1
            for s_ in shape[1:]:
                nel *= s_
            nb = nel * esz
            assert off % 4 == 0 and nb % 4 == 0, (off, shape)
            assert off + nb <= ARENA_BYTES, (off, nb)
            ap = arena[:, off // 4:(off + nb) // 4]
            if dt != F32:
                ap = ap.bitcast(dt)
            if len(shape) == 3:
                ap = ap.rearrange("p (a b) -> p a b", a=shape[1])
            return Tl(ap, ("sb", off, off + nb))

        class Bump:
            def __init__(self, lo, hi):
                self.o = lo
                self.hi = hi

            def __call__(self, shape, dt):
                esz = 4 if dt in (F32, I32) else 2
                nel = 1
                for s_ in shape[1:]:
                    nel *= s_
                nb = (nel * esz + 3) // 4 * 4
                t = sview(self.o, shape, dt)
                self.o += nb
                assert self.o <= self.hi, (self.o, self.hi)
                return t

        psi = [0]
        bpool = [list(range(8))]

        def bank():
            pool = bpool[0]
            b = pool[psi[0] % len(pool)]
            psi[0] += 1
            return Tl(ps[:, b, :], ("ps", b * 2048, (b + 1) * 2048))

        def cap(f, banks):
            old = bpool[0]
            bpool[0] = banks
            lst = P.capture(f)
            bpool[0] = old
            return lst

        def bf_view(bk, a, b):
            return bk.v(bk.ap.bitcast(BF16)[:, 0:a * b].rearrange("p (a b) -> p a b", a=a))

        def f_view(bk, a, b):
            return bk.v(bk.ap[:, 0:a * b].rearrange("p (a b) -> p a b", a=a))

        def mm(out, lhsT, rhs, start, stop):
            P.add("pe", lambda e, o=out.ap, l=lhsT.ap, r=rhs.ap, st=start, sp=stop:
                  e.matmul(o, l, r, start=st, stop=sp), reads=[lhsT, rhs], writes=[out])

        def tr(out, in_, ident):
            P.add("pe", lambda e, o=out.ap, i=in_.ap, d=ident.ap: e.transpose(o, i, d),
                  reads=[in_, ident], writes=[out])

        def act(out, in_, func, scale=1.0, bias=None):
            rd = [in_] + ([bias] if bias is not None else [])
            if bias is None:
                P.add("act", lambda e, o=out.ap, i=in_.ap, f=func, s=scale:
                      e.activation(o, i, f, scale=s), reads=rd, writes=[out])
            else:
                P.add("act", lambda e, o=out.ap, i=in_.ap, f=func, s=scale, b=bias.ap:
                      e.activation(o, i, f, bias=b, scale=s), reads=rd, writes=[out])

        def tt(out, a, b, op, eng="dve"):
            P.add(eng, lambda e, o=out.ap, x=a.ap, y=b.ap, p=op: e.tensor_tensor(o, x, y, p),
                  reads=[a, b], writes=[out])

        def ts(out, a, s1, s2, op0, op1=None, eng="dve"):
            rd = [a] + [s for s in (s1, s2) if isinstance(s, Tl)]
            v1 = s1.ap if isinstance(s1, Tl) else s1
            v2 = s2.ap if isinstance(s2, Tl) else s2
            if op1 is None:
                P.add(eng, lambda e, o=out.ap, x=a.ap, p=op0: e.tensor_scalar(o, x, v1, None, p),
                      reads=rd, writes=[out])
            else:
                P.add(eng, lambda e, o=out.ap, x=a.ap, p=op0, q=op1: e.tensor_scalar(o, x, v1, v2, p, q),
                      reads=rd, writes=[out])

        def stt(out, a, sc, b, op0, op1):
            rd = [a, b] + ([sc] if isinstance(sc, Tl) else [])
            sv = sc.ap if isinstance(sc, Tl) else sc
            P.add("dve", lambda e, o=out.ap, x=a.ap, y=b.ap, p=op0, q=op1:
                  e.scalar_tensor_tensor(o, x, sv, y, p, q), reads=rd, writes=[out])

        def red(out, in_, op):
            P.add("dve", lambda e, o=out.ap, i=in_.ap, p=op: e.tensor_reduce(o, i, AX.X, p),
                  reads=[in_], writes=[out])

        def recip(out, in_):
            P.add("dve", lambda e, o=out.ap, i=in_.ap: e.reciprocal(o, i), reads=[in_], writes=[out])

        def cp(out, in_, eng="dve"):
            if eng == "act":
                P.add("act", lambda e, o=out.ap, i=in_.ap: e.copy(o, i), reads=[in_], writes=[out])
            else:
                P.add(eng, lambda e, o=out.ap, i=in_.ap: e.tensor_copy(o, i), reads=[in_], writes=[out])

        def memset(out, val, eng="pool"):
            P.add(eng, lambda e, o=out.ap, v=val: e.memset(o, v), writes=[out])

        def dma(out, in_):
            return P.add("sp", lambda e, o=out.ap, i=in_.ap: e.dma_start(out=o, in_=i),
                         reads=[in_], writes=[out], dma=True)

        def idma(fn, reads, writes, grp="main"):
            return P.add("pool", fn, reads=reads, writes=writes, dma=True, grp=grp)

        def bc(t, shape, axis):
            return t.v(t.ap.unsqueeze(axis).to_broadcast(shape))

        CB = Bump(0, 11328)
        CPK = CB([128, NCPK], F32)
        CBF = CB([128, NCBF], BF16)
        X0 = 11328
        W0 = X0 + 65536
        S0 = W0 + 107648
        assert S0 == 184512

        def cslice(c0, n):
            return CPK[:, c0:c0 + n]

        GMIX = cslice(C_GMIX, 8)
        GFFN = cslice(C_GFFN, 8)
        PSC = cslice(C_PSC, 4)
        GHEAD = cslice(C_GHEAD, 4)
        CQ = CPK.v(CPK.ap[:, C_CQ:C_CQ + 16].rearrange("p (h j) -> p h j", h=4))
        CK = CPK.v(CPK.ap[:, C_CK:C_CK + 16].rearrange("p (h j) -> p h j", h=4))
        BIF = cslice(C_BIF, 8)
        BR = cslice(C_BR, 36)
        EPS_T = cslice(C_EPS, 1)
        ONE_T = cslice(C_ONE, 1)
        WR = CPK.v(CPK.ap[:, C_WR:C_WR + 288].rearrange("p (k n) -> p k n", k=8))
        IDF = cslice(C_IDF, 128)
        TRIF = cslice(C_TRI, 128)
        ONESF = cslice(C_ONES, 128)
        GFIN = cslice(C_GFIN, 1024)
        IDB = CBF[:, B_ID:B_ID + 128]
        PA = CBF.v(CBF.ap[:, B_PA:B_PA + 512].rearrange("p (g t) -> p g t", g=4))
        PB = CBF.v(CBF.ap[:, B_PB:B_PB + 512].rearrange("p (g t) -> p g t", g=4))
        PA0 = CBF.v(CBF.ap[:, B_PA0:B_PA0 + 512].rearrange("p (g t) -> p g t", g=4))
        TRIS = CBF[:, B_TRIS:B_TRIS + 128]
        ONESB = CBF[:, B_ONESB:B_ONESB + 128]
        GFFN2 = cslice(C_GFFN2, 8)
        PIDX2 = cslice(C_PIDX2, 1)
        THR16 = cslice(C_THR16, 16)
        THR48 = cslice(C_THR48, 48)

        dma(CPK, dreg("cpk", cpk_d, 0, 1))
        dma(CBF, dreg("cbf", cbf_d, 0, 1))

        WB = Bump(W0, S0)
        WIN = WB([128, 8, NIN], BF16)
        WPOOL = WB([128, 4, 128], BF16)
        WBRA = WB([128, 4, D], BF16)
        WBRB = WB([128, 4, D], BF16)
        WOUT = WB([128, 8, D], BF16)

        XB_ = Bump(X0, X0 + 65536)
        SBX = Bump(S0, ARENA_BYTES)
        XT = [XB_([128, D], F32) for _ in range(2)] + [SBX([128, D], F32), SBX([128, D], F32)]
        XS = XB_([128, D], BF16)
        HT_ = XB_([128, 8, 128], BF16)
        U = [XB_([128, 512], BF16) for _ in range(2)]
        VAUG = [XB_([128, 4, 130], BF16), SBX([128, 4, 130], BF16)]
        QC = XB_([128, 4, 132], F32)
        KC = XB_([128, 4, 132], F32)
        QACC = XB_([128, 4, 128], F32)
        KACC = XB_([128, 4, 128], F32)
        QT = [XB_([128, 4, 128], BF16), SBX([128, 4, 128], BF16)]
        KT = [XB_([128, 4, 128], BF16), SBX([128, 4, 128], BF16)]
        KP = [XB_([128, 4, 128], BF16), SBX([128, 4, 128], BF16)]
        SDT = XB_([128, 4, 128], BF16)
        C32 = XB_([128, 4, 129], F32)
        CBFS = XB_([128, 4, 130], BF16)
        SIGO = [XB_([128, 512], F32), SBX([128, 512], F32)]
        SGA = XB_([128, D], BF16)
        SGB = [XB_([128, D], BF16), SBX([128, D], BF16)]
        DT_ = XB_([128, 4, 128], BF16)
        YPT = XB_([128, 4, 128], BF16)
        HN = XB_([128, 4, 128], F32)
        HNO = XB_([128, 512], BF16)
        HMT = XB_([128, 4, 128], BF16)
        SMS = [XB_([128, 128], F32), SBX([128, 128], F32)]
        M1 = [XB_([128, D], F32), SBX([128, D], F32)]
        X1T = [XB_([128, D], F32), SBX([128, D], F32)]
        MRG = XB_([128, D], BF16)
        MT_ = XB_([128, 8, 128], BF16)
        TMPQK = sview(QACC.reg[1], [128, D], F32)
        assert KACC.reg[1] == QACC.reg[2]
        STG_N = 8
        STG = [sview(X0 + 65536 - (j + 1) * 4616, [128, 1154], F32) for j in range(STG_N)]

        class SmallSet:
            pass

        SMV = []
        for SM in SMS:
            smi = [0]

            def sm(n, SM=SM, smi=smi):
                t = SM[:, smi[0]:smi[0] + n]
                smi[0] += n
                assert smi[0] <= 128
                return t

            v_ = SmallSet()
            v_.SS, v_.SD_, v_.RSTD = sm(1), sm(1), sm(1)
            v_.GIF, v_.E1, v_.L1, v_.TMP4, v_.RR, v_.EB, v_.EBL = sm(8), sm(4), sm(4), sm(4), sm(4), sm(4), sm(4)
            (v_.NQV, v_.DEN, v_.RDEN, v_.FAC, v_.F2, v_.VH, v_.SDH, v_.RSH, v_.SC) = (sm(4) for _ in range(9))
            v_.ST6 = SM.v(sm(24).ap.rearrange("p (h s) -> p h s", h=4))
            v_.MV = SM.v(sm(8).ap.rearrange("p (h s) -> p h s", h=4))
            SMV.append(v_)

        cast_engs = ["act", "dve"]
        pcs = [0]

        def load_cast(dst, src_ap, src_reg, ncols):
            j = pcs[0]
            pcs[0] += 1
            st = STG[j % STG_N]
            stv = st[:, 0:ncols]
            dma(stv, dreg(*src_reg).v(src_ap))
            cp(dst, stv, eng=cast_engs[j % 2])

        for k in range(8):
            for c in range(4):
                load_cast(WIN[:, k, c * 1154:(c + 1) * 1154],
                          win_d[k * 128:(k + 1) * 128, c * 1154:(c + 1) * 1154],
                          ("w_in", win_d, 0, 1), 1154)
        st = STG[pcs[0] % STG_N]
        pcs[0] += 1
        stv = st.v(st.ap[:, 0:512].rearrange("p (g d) -> p g d", g=4))
        dma(stv, dreg("w_pool", wpool_d.rearrange("g c d -> c g d"), 0, 1))
        cp(WPOOL, stv, eng="dve")
        for g in range(4):
            load_cast(WBRA[:, g, :], wbra_d[g * 128:(g + 1) * 128, :], ("w_br_a", wbra_d, 0, 1), 1024)
        for g in range(4):
            load_cast(WBRB[:, g, :], wbrb_d[g * 128:(g + 1) * 128, :], ("w_br_b", wbrb_d, 0, 1), 1024)
        for k in range(8):
            load_cast(WOUT[:, k, :], wout_d[k * 128:(k + 1) * 128, :], ("w_out", wout_d, 0, 1), 1024)

        xs_flat = xs_h.ap().rearrange("(a b) f -> a (b f)", b=16)

        def zero_fill(c):
            dma(Tl(xs_flat[c * 128:(c + 1) * 128, :], ("dram:xs", 0, 64)), dreg("zx", zx_d, 0, 1))
        for b_ in range(2):
            memset(VAUG[b_], 1.0)
        memset(QC, 0.0)
        memset(KC, 0.0)
        memset(C32, 0.0)
        memset(CBFS, 0.0)

        LNS = math.log(DH ** -0.5)

        def xrow(i):
            return dreg("x", x_d[i * 128:(i + 1) * 128, :], i, i + 1)

        def x1row(i):
            return dreg("x1s", x1_d[i * 128:(i + 1) * 128, :], i, i + 1)

        def yrow(i):
            return dreg("y", y_d[i * 128:(i + 1) * 128, :], i, i + 1)

        def proj(col0, ncols):
            pb = bank()
            o = pb[:, 0:ncols]
            for k in range(8):
                mm(o, HT_[:, k, :], WIN[:, k, col0:col0 + ncols], k == 0, k == 7)
            return o

        def A1(i):
            b = i % 2
            xt = XT[i % 4]
            V = SMV[b]
            act(TMPQK, xt, AF.Square)
            red(V.SS, TMPQK, ALU.add)
            act(V.SD_, V.SS, AF.Sqrt, scale=1.0 / D, bias=EPS_T)
            recip(V.RSTD, V.SD_)
            ts(XS, xt, V.RSTD, None, ALU.mult)
            pt = bank()
            ptb = bf_view(pt, 8, 128)
            for k in range(8):
                tr(ptb[:, k, :], XS[:, k * 128:(k + 1) * 128], IDB)
            tt(HT_, ptb, bc(GMIX, [128, 8, 128], 2), ALU.mult)
            pu = proj(0, 512)
            cp(U[b], pu, eng="act")
            pv = proj(1536, 512)
            cp(VAUG[b][:, :, 0:128], pv.v(pv.ap.rearrange("p (h d) -> p h d", h=4)), eng="dve")
            po = proj(2048, 512)
            act(SIGO[b], po, AF.Sigmoid)
            pif = proj(2560, 8)
            tt(V.GIF, pif, BIF, ALU.add)
            act(V.E1, V.GIF[:, 4:8], AF.Exp, scale=-1.0)
            act(V.L1, V.E1, AF.Ln, bias=ONE_T)
            pgt = bank()
            mm(pgt[:, 0:4], TRIF, V.L1, True, True)
            mm(pgt[:, 4:8], ONESF, V.L1, True, True)
            stt(V.TMP4, V.GIF[:, 0:4], LNS, pgt[:, 0:4], ALU.add, ALU.add)
            act(V.RR, V.TMP4, AF.Exp)
            act(V.EB, pgt[:, 0:4], AF.Exp, scale=-1.0)
            act(V.EBL, pgt[:, 4:8], AF.Exp, scale=-1.0)

        def A2(i):
            b = i % 2
            ucur, uprev = U[b], U[(i + 1) % 2]
            for hf in range(2):
                pg_ = proj(2568 + hf * 512, 512)
                act(SGA[:, hf * 512:(hf + 1) * 512], pg_, AF.Sigmoid)
            for hf in range(2):
                pg_ = proj(3592 + hf * 512, 512)
                act(SGB[b][:, hf * 512:(hf + 1) * 512], pg_, AF.Sigmoid)
            pd = bank()
            pdv = f_view(pd, 4, 128)
            for g in range(4):
                mm(pdv[:, g, :], ucur[:, g * 128:(g + 1) * 128], (PA0 if i == 0 else PA)[:, g, :], True, i == 0)
                if i > 0:
                    mm(pdv[:, g, :], uprev[:, g * 128:(g + 1) * 128], PB[:, g, :], False, True)
            cp(DT_, pdv, eng="act")
            py = bank()
            pyv = f_view(py, 4, 128)
            for g in range(4):
                mm(pyv[:, g, :], WPOOL[:, g, :], DT_[:, g, :], True, True)
            tt(YPT, pyv, bc(PSC, [128, 4, 128], 2), ALU.mult)
            pa = [bank() for _ in range(2)]
            for hf in range(2):
                for g in range(4):
                    mm(pa[hf], YPT[:, g, :], WBRA[:, g, hf * 512:(hf + 1) * 512], g == 0, g == 3)
            for hf in range(2):
                sl = slice(hf * 512, (hf + 1) * 512)
                tt(M1[b][:, sl], pa[hf], SGA[:, sl], ALU.mult)

        def A3(i):
            b = i % 2
            V = SMV[b]
            for (col0, buf) in ((512, QC), (1024, KC)):
                pq = bank()
                pqv = f_view(pq, 4, 128)
                for h in range(4):
                    for k in range(8):
                        mm(pqv[:, h, :], WIN[:, k, col0 + h * 128:col0 + (h + 1) * 128], HT_[:, k, :],
                           k == 0, k == 7)
                cp(buf[:, :, 3:131], pqv, eng="act")
            TMPQ = TMPQK.v(TMPQK.ap[:, 0:512].rearrange("p (h t) -> p h t", h=4))
            TMPK = TMPQK.v(TMPQK.ap[:, 512:1024].rearrange("p (h t) -> p h t", h=4))
            for (buf, acc, cw, tmp, outT) in ((QC, QACC, CQ, TMPQ, QT[b]), (KC, KACC, CK, TMPK, KT[b])):
                for j in range(4):
                    for h in range(4):
                        ah = acc.chunk(h, 4)
                        if j == 0:
                            ts(ah, buf[:, h, 0:128], cw[:, h, 0:1], None, ALU.mult)
                        else:
                            stt(ah, buf[:, h, j:j + 128], cw[:, h, j:j + 1], ah, ALU.mult, ALU.add)
                act(outT, acc, AF.Silu)
                cp(buf[:, :, 0:3], buf[:, :, 128:131], eng="dve")
            pk = bank()
            pkb = bf_view(pk, 4, 128)
            for h in range(4):
                tr(pkb[:, h, :], KT[b][:, h, :], IDB)
            tt(KP[b], pkb, bc(V.RR, [128, 4, 128], 2), ALU.mult)

        def B1(i):
            b = i % 2
            V = SMV[b]
            psb = bank()
            psv = f_view(psb, 4, 128)
            for h in range(4):
                mm(psv[:, h, :], KT[b][:, h, :], QT[b][:, h, :], True, True)
            for h in range(4):
                stt(SDT[:, h, :], psv[:, h, :], V.RR[:, h:h + 1], TRIF, ALU.mult, ALU.mult)
            pn = [f_view(bank(), 2, 129) for _ in range(2)]
            for h in range(4):
                o = pn[h // 2][:, h % 2, :]
                mm(o, SDT[:, h, :], VAUG[b][:, h, 0:129], True, i == 0)
                if i > 0:
                    mm(o, QT[b][:, h, :], CBFS[:, h, 0:129], False, True)
            if i + 1 < NT:
                pc = [f_view(bank(), 2, 129) for _ in range(2)]
                for h in range(4):
                    mm(pc[h // 2][:, h % 2, :], KP[b][:, h, :], VAUG[b][:, h, 0:129], True, True)
            for p_ in range(2):
                tt(V.NQV[:, 2 * p_:2 * p_ + 2], pn[p_][:, :, 128], V.EB[:, 2 * p_:2 * p_ + 2], ALU.mult)
            ts(V.RDEN, V.NQV, -1.0, None, ALU.mult)
            tt(V.DEN, V.NQV, V.RDEN, ALU.max)
            ts(V.DEN, V.DEN, 1.0, None, ALU.max)
            recip(V.RDEN, V.DEN)
            tt(V.FAC, V.EB, V.RDEN, ALU.mult)
            for h in range(4):
                P.add("dve", lambda e, o=V.ST6.ap[:, h, :], i_=pn[h // 2].ap[:, h % 2, 0:128]: e.bn_stats(o, i_),
                      reads=[pn[h // 2]], writes=[V.ST6])
                P.add("dve", lambda e, o=V.MV.ap[:, h, :], i_=V.ST6.ap[:, h, :]: e.bn_aggr(o, i_),
                      reads=[V.ST6], writes=[V.MV])
            tt(V.F2, V.FAC, V.FAC, ALU.mult)
            tt(V.VH, V.F2, V.MV[:, :, 1], ALU.mult)
            act(V.SDH, V.VH, AF.Sqrt, bias=EPS_T)
            recip(V.RSH, V.SDH)
            tt(V.SC, V.FAC, V.RSH, ALU.mult)
            for h in range(4):
                ts(HN[:, h, :], pn[h // 2][:, h % 2, 0:128], V.MV[:, h, 0:1], V.SC[:, h:h + 1],
                   ALU.subtract, ALU.mult)
            tt(HNO, HN.v(HN.ap.rearrange("p h d -> p (h d)")), SIGO[b], ALU.mult)
            if i + 1 < NT:
                for p_ in range(2):
                    tt(C32[:, 2 * p_:2 * p_ + 2, :], C32[:, 2 * p_:2 * p_ + 2, :], pc[p_], ALU.add)
                    tt(C32[:, 2 * p_:2 * p_ + 2, :], C32[:, 2 * p_:2 * p_ + 2, :],
                       V.EBL.v(V.EBL.ap[:, 2 * p_:2 * p_ + 2].unsqueeze(2).to_broadcast([128, 2, 129])), ALU.mult)
                cp(CBFS[:, :, 0:129], C32, eng="act")

        def B2a(i):
            b = i % 2
            ph = bank()
            phb = bf_view(ph, 4, 128)
            for h in range(4):
                tr(phb[:, h, :], HNO[:, h * 128:(h + 1) * 128], IDB)
            tt(HMT, phb, bc(GHEAD, [128, 4, 128], 2), ALU.mult)
            pbb = [bank() for _ in range(2)]
            for hf in range(2):
                for h in range(4):
                    mm(pbb[hf], HMT[:, h, :], WBRB[:, h, hf * 512:(hf + 1) * 512], h == 0, h == 3)
            for hf in range(2):
                sl = slice(hf * 512, (hf + 1) * 512)
                tt(X1T[b][:, sl], pbb[hf], SGB[b][:, sl], ALU.mult)

        def B2b(i):
            b = i % 2
            xt = XT[i % 4]
            tt(MRG, M1[b], X1T[b], ALU.add)
            pm = bank()
            pmb = bf_view(pm, 8, 128)
            for k in range(8):
                tr(pmb[:, k, :], MRG[:, k * 128:(k + 1) * 128], IDB)
            cp(MT_, pmb, eng="act")
            pox = [bank() for _ in range(2)]
            for hf in range(2):
                for k in range(8):
                    mm(pox[hf], MT_[:, k, :], WOUT[:, k, hf * 512:(hf + 1) * 512], k == 0, k == 7)
            for hf in range(2):
                sl = slice(hf * 512, (hf + 1) * 512)
                tt(X1T[b][:, sl], pox[hf], xt[:, sl], ALU.add)
            dma(yrow(i) if STAGE == "mixer" else x1row(i), X1T[b])
            if i + 4 < NT:
                dma(XT[i % 4], xrow(i + 4))
            if STAGE != "mixer" and i % 2 == 1 and i // 2 < NROWS // 16 // 128:
                zero_fill(i // 2)

        dma(XT[0], xrow(0))
        dma(XT[1], xrow(1))
        dma(XT[2], xrow(2))
        dma(XT[3], xrow(3))
        WSRC = (weg_h, weu_h, wed_h)
        NCV = 32

        def conv_w(n_, gate=None):
            m_, c = divmod(n_, NCV)
            if m_ < 2:
                r0, r1 = c * 8192 // NCV, (c + 1) * 8192 // NCV
                dst_ap = wb_h[m_].ap().rearrange("r (h m) -> (r h) m", h=2)[r0:r1, :]
                src_ap = WSRC[m_][r0:r1, :]
            else:
                assert NCV == 32
                src_ap = WSRC[2].ap().rearrange("r (h m) -> (r h) m", h=2)[c * 512:(c + 1) * 512, :].rearrange(
                    "(k p) n -> p k n", p=128)
                dst_ap = wb_h[2].ap().rearrange("(e p) (k n) -> e p k n", p=128, k=4)[c]
            rd = [Tl(None, ("fake:cv", n_ - 2, n_ - 1))] if n_ >= 2 else []
            if gate is not None:
                rd.append(gate)
            idma(lambda e, o=dst_ap, src=src_ap: e.dma_start(out=o, in_=src),
                 reads=rd, writes=[Tl(None, ("dram:wb%d" % m_, c, c + 1)), Tl(None, ("fake:cv", n_, n_ + 1))],
                 grp="cv")
        A1(0)
        if STAGE != "mixer":
            for n_ in range(3 * NCV):
                conv_w(n_, gate=(XS if n_ == 0 else None))
        P.merge([cap(lambda: A3(0), [0, 1, 2, 3]), cap(lambda: A2(0), [4, 5, 6, 7])])
        for i in range(NT):
            nx = i + 1 < NT
            th = [cap(lambda: B1(i), [0, 1, 2, 3, 4])]
            if nx:
                th.append(cap(lambda: A1(i + 1), [5, 6, 7]))
            P.merge(th)
            th = [cap(lambda: (B2a(i), B2b(i)), [0, 1, 2])]
            if nx:
                th.append(cap(lambda: A3(i + 1), [3, 4]))
                th.append(cap(lambda: A2(i + 1), [5, 6, 7]))
            P.merge(th)

        out_dmas = []
        if STAGE != "mixer":
            def xres(i, hf=None):
                if hf is None:
                    return sview(X0 + i * 4096, [128, D], F32)
                return sview(X0 + i * 4096 + hf * 2048, [128, 512], F32)

            MB = Bump(W0, S0)
            XSB = MB([128, NT, D], BF16)
            WG = [MB([128, 8, 512], BF16) for _ in range(2)]
            WU = [MB([128, 8, 512], BF16) for _ in range(2)]
            WD = [MB([128, 4, D], BF16) for _ in range(2)]
            ESTG = [MB([128, 2048], F32) for _ in range(3)]

            def sv(off, shape, dt):
                return sview(S0 + off, shape, dt)

            DESTI = sv(0, [128, 32], I32)
            GK = sv(128, [128, 32], F32)
            IDXI = sv(256, [128, NBLK], I32)
            E0 = ESTG[0].reg[1]
            RSA = [sv(640, [128, 160], F32), sv(1280, [128, 160], F32),
                   sview(E0 + 16384, [128, 160], F32), sview(E0 + 17024, [128, 160], F32)]
            RS = sv(1920, [128, 416], F32)
            OHK = sv(3584, [128, NT * 2 * 32], F32)
            OHSB = sv(7680, [128, NT, 32], BF16)
            RANK = sv(8704, [128, NT, 32], F32)
            XS32 = [sv(10752, [128, D], F32), sv(18944, [128, D], F32),
                    sview(E0, [128, D], F32), sview(E0 + 8192, [128, D], F32)]
            HF32 = [sv(14848, [128, 8, 128], F32), sv(23040, [128, 8, 128], F32),
                    sview(E0 + 4096, [128, 8, 128], F32), sview(E0 + 12288, [128, 8, 128], F32)]
            TMPD = sv(10752, [128, NT * 2 * 32], F32)
            TMPB = sv(14848, [128, NBLK * 32], F32)
            NB16 = sv(20992, [128, 32 * 16], F32)
            XB = sv(3584, [128, 2, D], BF16)
            XTB = sv(7680, [128, 8, 256], BF16)
            HTB = [sv(11776, [128, 4, 256], BF16), sv(13824, [128, 4, 256], BF16)]
            SGb = [sv(15872, [128, 256], F32), sv(16896, [128, 256], F32)]
            YB = sv(17920, [128, 2, D], F32)
            YG = [sv(3584 + n_ * 4096, [128, D], F32) for n_ in range(3)]
            OUTT = [sv(15872, [128, D], F32), sv(19968, [128, D], F32)]
            assert S0 + 27136 <= ARENA_BYTES
            rsi = [0]

            def rs(n):
                t = RS[:, rsi[0]:rsi[0] + n]
                rsi[0] += n
                assert rsi[0] <= 416
                return t

            class RT:
                pass

            RTS = []
            for RSx in RSA:
                c_ = [0]

                def ra(n, RSx=RSx, c_=c_):
                    t = RSx[:, c_[0]:c_[0] + n]
                    c_[0] += n
                    assert c_[0] <= 160
                    return t

                r_ = RT()
                r_.SS2, r_.SD2, r_.RSTD2 = ra(1), ra(1), ra(1)
                r_.LG = ra(36)
                r_.GMAX, r_.OHG, r_.GSH, r_.GE, r_.GSUM, r_.PGV = ra(1), ra(4), ra(4), ra(4), ra(1), ra(1)
                r_.T32 = RSx.v(ra(32).ap.rearrange("p (g j) -> p g j", g=4))
                r_.ESEL, r_.M1_, r_.OH1, r_.MSK, r_.M2_, r_.OH2 = ra(8), ra(1), ra(8), ra(8), ra(1), ra(8)
                r_.DD, r_.E2, r_.DEN2, r_.W1, r_.W2 = (ra(1) for _ in range(5))
                RTS.append(r_)
            SS2, SD2, RSTD2 = RTS[0].SS2, RTS[0].SD2, RTS[0].RSTD2
            CUM, NBLKT, PADA, CSA, CSB_, POFF = (rs(32) for _ in range(6))
            BEF, EMP, IDXF, IDXF1 = (rs(NBLK) for _ in range(4))
            DESTF = rs(32)
            ohk4 = OHK.v(OHK.ap.rearrange("p (i k e) -> p i k e", i=NT, k=2))

            def sub(t, ap, off, nb):
                sp_, s0_, _ = t.reg
                return Tl(ap, (sp_, s0_ + off, s0_ + off + nb))

            for i in range(NT):
                dma(xres(i), x1row(i))

            def route(i, th):
                R = RTS[th]
                xs32, hf32 = XS32[th], HF32[th]
                sqs = hf32.v(hf32.ap.rearrange("p k t -> p (k t)"))
                xi = xres(i)
                act(sqs, xi, AF.Square)
                red(R.SS2, sqs, ALU.add)
                act(R.SD2, R.SS2, AF.Sqrt, scale=1.0 / D, bias=EPS_T)
                recip(R.RSTD2, R.SD2)
                ts(xs32, xi, R.RSTD2, None, ALU.mult)
                cp(sub(XSB, XSB.ap[:, i, :], i * 2048, 2048), xs32, eng="pool")
                pp = [bank() for _ in range(2)]
                for k in range(8):
                    tr(pp[k // 4][:, (k % 4) * 128:(k % 4 + 1) * 128], xs32[:, k * 128:(k + 1) * 128], IDF)
                for hf in range(2):
                    tt(hf32[:, 4 * hf:4 * hf + 4, :], f_view(pp[hf], 4, 128),
                       GFFN.v(GFFN.ap[:, 4 * hf:4 * hf + 4].unsqueeze(2).to_broadcast([128, 4, 128])), ALU.mult)
                pr = bank()
                for k in range(8):
                    mm(pr[:, 0:36], hf32[:, k, :], WR[:, k, :], k == 0, k == 7)
                lg4 = LG4B[(i // 4) % 2]
                tt(sub(lg4, lg4.ap[:, th, :], th * 144, 144), pr[:, 0:36], BR, ALU.add)

            BT = sview(E0 + 17664, [128, 768], F32)
            bti = [0]

            def bt(n, shape=None):
                t = BT[:, bti[0]:bti[0] + n]
                bti[0] += n
                assert bti[0] <= 768
                if shape is not None:
                    t = t.v(t.ap.rearrange("p (a b) -> p a b", a=shape[0]))
                return t

            LG4B = [bt(144, (4, 36)), bt(144, (4, 36))]
            GMAX4, GSUM4, PGV4, M14, M24, DD4, E24, DEN4, W14, W24 = (bt(4) for _ in range(10))
            OHG4, GSH4, GE4 = bt(16, (4, 4)), bt(16, (4, 4)), bt(16, (4, 4))
            T324 = bt(128)
            ESEL4, OH14, MSK4, OH24 = (bt(32, (4, 8)) for _ in range(4))

            def b3(t, shape):
                return t.v(t.ap.unsqueeze(2).to_broadcast(shape))

            def route_small(g0):
                LG4 = LG4B[(g0 // 4) % 2]
                gl = LG4[:, :, 0:4]
                el = LG4.v(LG4.ap[:, :, 4:36].rearrange("p t (g j) -> p t g j", g=4))
                red(GMAX4, gl, ALU.max)
                tt(OHG4, gl, b3(GMAX4, [128, 4, 4]), ALU.is_equal)
                tt(GSH4, gl, b3(GMAX4, [128, 4, 4]), ALU.subtract)
                act(GE4, GSH4, AF.Exp)
                red(GSUM4, GE4, ALU.add)
                recip(PGV4, GSUM4)
                t4 = T324.v(T324.ap.rearrange("p (t g j) -> p t g j", t=4, g=4))
                tt(t4, el, OHG4.v(OHG4.ap.unsqueeze(3).to_broadcast([128, 4, 4, 8])), ALU.mult)
                red(ESEL4, T324.v(T324.ap.rearrange("p (t g j) -> p t j g", t=4, g=4)), ALU.add)
                red(M14, ESEL4, ALU.max)
                tt(OH14, ESEL4, b3(M14, [128, 4, 8]), ALU.is_equal)
                stt(MSK4, OH14, -1e30, ESEL4, ALU.mult, ALU.add)
                red(M24, MSK4, ALU.max)
                tt(OH24, MSK4, b3(M24, [128, 4, 8]), ALU.is_equal)
                tt(DD4, M24, M14, ALU.subtract)
                act(E24, DD4, AF.Exp)
                ts(DEN4, E24, 1.0, None, ALU.add)
                recip(W14, DEN4)
                tt(W24, E24, W14, ALU.mult)
                gk4 = GK.v(GK.ap[:, 2 * g0:2 * g0 + 8].rearrange("p (t k) -> p t k", k=2))
                tt(gk4[:, :, 0], PGV4, W14, ALU.mult)
                tt(gk4[:, :, 1], PGV4, W24, ALU.mult)
                ohg_b = OHG4.v(OHG4.ap.unsqueeze(3).to_broadcast([128, 4, 4, 8]))
                for k_, oh in ((0, OH14), (1, OH24)):
                    dst = OHK.v(ohk4.ap[:, g0:g0 + 4, k_, :].rearrange("p t (g j) -> p t g j", g=4))
                    tt(dst, ohg_b, oh.v(oh.ap.unsqueeze(2).to_broadcast([128, 4, 4, 8])), ALU.mult)
                tt(OHSB[:, g0:g0 + 4, :], ohk4[:, g0:g0 + 4, 0, :], ohk4[:, g0:g0 + 4, 1, :], ALU.add)

            for i in range(0, NT, 4):
                th = [cap(lambda: route(i + t_, t_), [2 * t_, 2 * t_ + 1]) for t_ in range(4)]
                if i >= 4:
                    th.append(cap(lambda: route_small(i - 4), [0]))
                P.merge(th)
            route_small(NT - 4)

            prk = bank()
            pct = bank()
            for i in range(NT):
                o = prk[:, i * 32:(i + 1) * 32]
                mm(o, TRIS, OHSB[:, i, :], True, i == 0)
                for i2 in range(i):
                    mm(o, ONESB, OHSB[:, i2, :], False, i2 == i - 1)
            for i in range(NT):
                mm(pct[:, 0:32], ONESB, OHSB[:, i, :], i == 0, i == NT - 1)
            cp(CUM, pct[:, 0:32])
            nb16 = NB16.v(NB16.ap.rearrange("p (e m) -> p e m", m=16))
            tt(nb16, CUM.v(CUM.ap.unsqueeze(2).to_broadcast([128, 32, 16])),
               THR16.v(THR16.ap.unsqueeze(1).to_broadcast([128, 32, 16])), ALU.is_gt)
            red(NBLKT, nb16, ALU.add)
            ts(PADA, NBLKT, 256.0, None, ALU.mult)
            cp(CSA, PADA)
            cur, nxt = CSA, CSB_
            for sh in (1, 2, 4, 8, 16):
                cp(nxt[:, 0:sh], cur[:, 0:sh])
                tt(nxt[:, sh:32], cur[:, sh:32], cur[:, 0:32 - sh], ALU.add)
                cur, nxt = nxt, cur
            PEND = cur
            tt(POFF, PEND, PADA, ALU.subtract)
            tt(RANK, prk.v(prk.ap.rearrange("p (i e) -> p i e", e=32)),
               POFF.v(POFF.ap.unsqueeze(1).to_broadcast([128, NT, 32])), ALU.add)
            tmpd4 = TMPD.v(TMPD.ap.rearrange("p (i k e) -> p i k e", i=NT, k=2))
            tt(tmpd4, ohk4, RANK.v(RANK.ap.unsqueeze(2).to_broadcast([128, NT, 2, 32])), ALU.mult)
            red(DESTF, TMPD.v(TMPD.ap.rearrange("p (n e) -> p n e", e=32)), ALU.add)
            cp(DESTI, DESTF)
            tmpb3 = TMPB.v(TMPB.ap.rearrange("p (j e) -> p j e", e=32))
            tt(tmpb3, PEND.v(PEND.ap.unsqueeze(1).to_broadcast([128, NBLK, 32])),
               THR48.v(THR48.ap.unsqueeze(2).to_broadcast([128, NBLK, 32])), ALU.is_le)
            red(BEF, tmpb3, ALU.add)
            ts(BEF, BEF, 31.0, None, ALU.min)
            ts(EMP, THR48, PEND[:, 31:32], None, ALU.is_ge)
            ts(IDXF, BEF, 256.0, PIDX2, ALU.mult, ALU.add)
            ts(IDXF, IDXF, 0.5, None, ALU.mult)
            stt(IDXF, EMP, float(1 << 21), IDXF, ALU.mult, ALU.add)
            cp(IDXI, IDXF)

            IOA = bass.IndirectOffsetOnAxis
            regcache = {}

            def breg(e, v):
                if v not in regcache:
                    regcache[v] = e.to_reg(v)
                return regcache[v]


            XSB_OFF = XSB.reg[1]
            XB2 = [XB, sview(XSB_OFF + 24576, [128, 2, D], BF16)]
            XTB2 = [XTB, sview(XSB_OFF + 28672, [128, 8, 256], BF16)]

            def gather_w(j, m_):
                dT = (WG, WU, WD)[m_][j % 2]
                dflat = dT.v(dT.ap.rearrange("p k n -> p (k n)"))
                idma(lambda e, o=dflat.ap, src=wb_h[m_][:, :], idx=IDXI.ap[:, j:j + 1]:
                     e.indirect_dma_start(out=o, out_offset=None, in_=src, in_offset=IOA(ap=idx, axis=0),
                                          bounds_check=breg(e, 4095), oob_is_err=False),
                     reads=[IDXI, Tl(None, ("dram:wb%d" % m_, 0, 64))], writes=[dflat])

            def load_x(j):
                dma(XB2[j % 2], Tl(xs_h.ap()[j * 256:(j + 1) * 256, :].rearrange("(r p) f -> p r f", p=128),
                                   ("dram:xs", 0, 64)))

            def blk_B1(j):
                xb = XB2[j % 2]
                xtb = XTB2[j % 2]
                for r in range(2):
                    pt = bank()
                    ptb = bf_view(pt, 8, 128)
                    xr_ = xb.v(xb.ap[:, r, :].rearrange("p (c k) -> p k c", k=8))
                    for k in range(8):
                        tr(ptb[:, k, :], xr_[:, k, :], IDB)
                    tt(xtb[:, :, r * 128:(r + 1) * 128], ptb, bc(GFFN2, [128, 8, 128], 2), ALU.mult)

            sgi = [0]

            def blk_B2(j, hcs):
                b = j % 2
                htb = HTB[j % 2]
                xtb = XTB2[j % 2]
                for hc in hcs:
                    pg = bank()
                    pu = bank()
                    for k in range(8):
                        mm(pg[:, 0:256], WG[b][:, k, hc * 128:(hc + 1) * 128], xtb[:, k, :], k == 0, k == 7)
                    for k in range(8):
                        mm(pu[:, 0:256], WU[b][:, k, hc * 128:(hc + 1) * 128], xtb[:, k, :], k == 0, k == 7)
                    sg = SGb[sgi[0] % 2]
                    sgi[0] += 1
                    act(sg, pg[:, 0:256], AF.Silu)
                    tt(htb[:, hc, :], sg, pu[:, 0:256], ALU.mult)

            def blk_B3(j):
                b = j % 2
                htb = HTB[j % 2]
                for r in range(2):
                    for hf in range(2):
                        py_ = bank()
                        for hc in range(4):
                            mm(py_, htb[:, hc, r * 128:(r + 1) * 128], WD[b][:, hc, hf * 512:(hf + 1) * 512],
                               hc == 0, hc == 3)
                        cp(YB[:, r, hf * 512:(hf + 1) * 512], py_, eng=("act" if (r + hf) % 2 == 0 else "dve"))
                dma(Tl(ys_h.ap()[j * 256:(j + 1) * 256, :].rearrange("(r p) f -> p r f", p=128),
                       ("dram:ys", j, j + 1)), YB)

            nblk_run = NBLK
            for m_ in range(3):
                gather_w(0, m_)
            for i in range(NT):
                for k_ in range(2):
                    n_ = 2 * i + k_
                    idma(lambda e, o=xs_h[:, :], idx=DESTI.ap[:, n_:n_ + 1], src=XSB.ap[:, i, :]:
                         e.indirect_dma_start(out=o, out_offset=IOA(ap=idx, axis=0), in_=src, in_offset=None,
                                              bounds_check=breg(e, NROWS - 1), oob_is_err=False),
                         reads=[XSB, DESTI], writes=[Tl(None, ("dram:xs", n_, n_ + 1))])

            load_x(0)
            blk_B1(0)
            load_x(1)
            for j in range(nblk_run):
                nx = j + 1 < nblk_run
                if nx:
                    for m_ in range(3):
                        gather_w(j + 1, m_)

                def t_main(j=j):
                    blk_B2(j, (0, 1, 2, 3))
                    blk_B3(j)

                def t_next(j=j):
                    blk_B1(j + 1)
                    if j + 2 < nblk_run:
                        load_x(j + 2)

                th = [cap(t_main, [0, 1, 2, 3, 4, 5])]
                if nx:
                    th.append(cap(t_next, [6, 7]))
                P.merge(th)

            YGR = YG + [sview(E0 + n_ * 4096, [128, D], F32) for n_ in range(6)]
            NYG = len(YGR)

            def fin(i, th):
                R = RTS[th]
                xr = xres(i)
                for k_ in range(2):
                    n_ = 2 * i + k_
                    yg = YGR[n_ % NYG]
                    idma(lambda e, o=yg.ap, idx=DESTI.ap[:, n_:n_ + 1]:
                         e.indirect_dma_start(out=o, out_offset=None, in_=ys_h[:, :], in_offset=IOA(ap=idx, axis=0),
                                              bounds_check=breg(e, NROWS - 1), oob_is_err=False),
                         reads=[DESTI, Tl(None, ("dram:ys", 0, 64))], writes=[yg])
                    stt(xr, yg, GK[:, n_:n_ + 1], xr, ALU.mult, ALU.add)
                sq_ = YGR[(2 * i) % NYG]
                ot = OUTT[i % 2]
                act(sq_, xr, AF.Square)
                red(R.SS2, sq_, ALU.add)
                act(R.SD2, R.SS2, AF.Sqrt, scale=1.0 / D, bias=EPS_T)
                recip(R.RSTD2, R.SD2)
                stt(ot, xr, R.RSTD2, GFIN, ALU.mult, ALU.mult)
                out_dmas.append(dma(yrow(i), ot))

            for i in range(0, NT, 2):
                P.merge([cap(lambda: fin(i, 0), [0]), cap(lambda: fin(i + 1, 1), [1])])
        sink = P.add("sp", None)
        for i_, o in enumerate(P.ops):
            if o["dma"] and o["fn"] is not None:
                P.ops[sink]["deps"].add(i_)

        n_eng_sems = 4
        from contextlib import ExitStack
        with ExitStack() as es:
            eng_sems = {e: [es.enter_context(nc.semaphore(f"s_{e}{j}")) for j in range(n_eng_sems)]
                        for e in ("pe", "act", "dve", "pool")}
            dma_sems = {"main": [es.enter_context(nc.semaphore(f"s_dma{j}")) for j in range(24)],
                        "cv": [es.enter_context(nc.semaphore(f"s_cv{j}")) for j in range(4)]}
            block = es.enter_context(nc.Block())
            P.emit(nc, block, eng_sems, dma_sems)
    return nc, P


def _consts():
    tri = np.triu(np.ones((128, 128), np.float32))
    pa = np.zeros((4, 128, 128), np.float32)
    pb = np.zeros((4, 128, 128), np.float32)
    pa0 = np.zeros((4, 128, 128), np.float32)
    for g, w in enumerate(POOL_WINDOWS):
        for t in range(128):
            for s in range(max(t - w + 1, -128), t + 1):
                if s >= 0:
                    pa[g, s, t] += 1.0 / w
                else:
                    pb[g, s + 128, t] += 1.0 / w
            pa[g, t, t] -= 1.0
            lo = max(t - w + 1, 0)
            cnt = t + 1 - lo
            for s in range(lo, t + 1):
                pa0[g, s, t] += 1.0 / cnt
            pa0[g, t, t] -= 1.0
    cbf = np.zeros((128, NCBF), np.float32)
    cbf[:, B_ID:B_ID + 128] = np.eye(128, dtype=np.float32)
    cbf[:, B_PA:B_PA + 512] = pa.transpose(1, 0, 2).reshape(128, 512)
    cbf[:, B_PB:B_PB + 512] = pb.transpose(1, 0, 2).reshape(128, 512)
    cbf[:, B_PA0:B_PA0 + 512] = pa0.transpose(1, 0, 2).reshape(128, 512)
    cbf[:, B_TRIS:B_TRIS + 128] = np.triu(np.ones((128, 128), np.float32), 1)
    cbf[:, B_ONESB:B_ONESB + 128] = 1.0
    return tri, cbf.astype(ml_dtypes.bfloat16)


_CACHE = {}


def kernel(x, g_mix, w_in, b_if, conv_q, conv_k, g_head, w_pool, pool_scale, w_br_a, w_br_b,
           w_out, g_ffn, w_rg, b_rg, w_re, b_re, w_e_gate, w_e_up, w_e_down, g_final):
    f = np.float32
    x = np.asarray(x, f)
    tri, cbf = _consts()
    cpk = np.zeros((128, NCPK), f)
    cpk[:, C_GMIX:C_GMIX + 8] = np.asarray(g_mix, f)[0].reshape(8, 128).T
    cpk[:, C_GFFN:C_GFFN + 8] = np.asarray(g_ffn, f)[0].reshape(8, 128).T
    cpk[:, C_PSC:C_PSC + 4] = np.asarray(pool_scale, f)[0].reshape(4, 128).T
    cpk[:, C_GHEAD:C_GHEAD + 4] = np.asarray(g_head, f)[0].reshape(4, 128).T
    cpk[:, C_CQ:C_CQ + 16] = np.asarray(conv_q, f)[0].reshape(4, 4, 128).transpose(2, 1, 0).reshape(128, 16)
    cpk[:, C_CK:C_CK + 16] = np.asarray(conv_k, f)[0].reshape(4, 4, 128).transpose(2, 1, 0).reshape(128, 16)
    cpk[:, C_BIF:C_BIF + 8] = np.asarray(b_if, f)[0][None, :]
    cpk[:, C_BR:C_BR + 4] = np.asarray(b_rg, f)[0][None, :]
    cpk[:, C_BR + 4:C_BR + 36] = np.asarray(b_re, f)[0][None, :]
    cpk[:, C_EPS] = EPS
    cpk[:, C_ONE] = 1.0
    wr = np.concatenate([np.asarray(w_rg, f)[0], np.asarray(w_re, f)[0]], axis=1)
    cpk[:, C_WR:C_WR + 288] = wr.reshape(8, 128, 36).transpose(1, 0, 2).reshape(128, 288)
    cpk[:, C_IDF:C_IDF + 128] = np.eye(128, dtype=f)
    cpk[:, C_TRI:C_TRI + 128] = tri
    cpk[:, C_ONES:C_ONES + 128] = 1.0
    cpk[:, C_GFIN:C_GFIN + 1024] = np.asarray(g_final, f)[None, :]
    cpk[:, C_GFFN2:C_GFFN2 + 8] = np.asarray(g_ffn, f)[0].reshape(128, 8)
    cpk[:, C_PIDX2] = 2.0 * np.arange(128, dtype=f)
    cpk[:, C_THR16:C_THR16 + 16] = 256.0 * np.arange(16, dtype=f)[None, :]
    cpk[:, C_THR48:C_THR48 + 48] = 256.0 * np.arange(48, dtype=f)[None, :]

    key = (STAGE, N_EXPERTS_RUN)
    if key not in _CACHE:
        _CACHE[key] = build_nc()[0]
    nc = _CACHE[key]
    shared = {
        "w_in": np.ascontiguousarray(np.asarray(w_in, f)[0]),
        "w_pool": np.ascontiguousarray(np.asarray(w_pool, f)[0]),
        "w_br_a": np.ascontiguousarray(np.asarray(w_br_a, f)[0]),
        "w_br_b": np.ascontiguousarray(np.asarray(w_br_b, f)[0]),
        "w_out": np.ascontiguousarray(np.asarray(w_out, f)[0]),
        "w_e_gate": np.ascontiguousarray(np.asarray(w_e_gate, f)[0]).reshape(8192, 2048),
        "w_e_up": np.ascontiguousarray(np.asarray(w_e_up, f)[0]).reshape(8192, 2048),
        "w_e_down": np.ascontiguousarray(np.asarray(w_e_down, f)[0]).reshape(8192, 2048),
        "zx": np.zeros((128, 16384), ml_dtypes.bfloat16),
        "cpk": cpk,
        "cbf": cbf,
    }
    in_maps = []
    for c in range(8):
        m = dict(shared)
        m["x"] = np.ascontiguousarray(x[c])
        in_maps.append(m)
    res = run_bass_kernel_spmd(nc, in_maps, core_ids=list(range(8)))
    return np.stack([np.asarray(r["y"], f) for r in res.results], axis=0)
```

```python
import math
import numpy as np
import ml_dtypes
import concourse.bass as bass
import concourse.mybir as mybir
from concourse.bass_utils import run_bass_kernel_spmd

F32 = mybir.dt.float32
BF16 = mybir.dt.bfloat16
I32 = mybir.dt.int32
AF = mybir.ActivationFunctionType
ALU = mybir.AluOpType
AX = mybir.AxisListType

D = 1024
S = 2048
NT = S // 128
NIN = 4616
NE = 32
DH = 128
EPS = 1e-6
POOL_WINDOWS = (2, 4, 8, 16)
STAGE = "full"
N_EXPERTS_RUN = NE

C_GMIX, C_GFFN, C_PSC, C_GHEAD, C_CQ, C_CK, C_BIF, C_BR, C_EPS, C_ONE = 0, 8, 16, 20, 24, 40, 56, 64, 100, 101
C_WR = 102
C_IDF = C_WR + 8 * 36
C_TRI = C_IDF + 128
C_ONES = C_TRI + 128
C_GFIN = C_ONES + 128
C_GFFN2 = C_GFIN + 1024
C_PIDX2 = C_GFFN2 + 8
C_THR16 = C_PIDX2 + 1
C_THR48 = C_THR16 + 16
NCPK = C_THR48 + 48
B_ID, B_PA, B_PB, B_PA0 = 0, 128, 640, 1152
B_TRIS, B_ONESB = 1664, 1792
NCBF = 1920
NBLK = 48
NROWS = NBLK * 256


class Tl:
    __slots__ = ("ap", "reg")

    def __init__(self, ap, reg):
        self.ap = ap
        self.reg = reg

    def __getitem__(self, idx):
        return Tl(self.ap[idx], self.reg)

    def v(self, ap):
        return Tl(ap, self.reg)

    def chunk(self, i, n):
        sp, s0, e0 = self.reg
        w = (e0 - s0) // n
        return Tl(self.ap[:, i], (sp, s0 + i * w, s0 + (i + 1) * w))


class Prog:
    LIMIT = 16000

    def __init__(self):
        self.ops = []
        self.recs = {}
        self._cap = None

    def capture(self, f):
        self._cap = []
        f()
        lst = self._cap
        self._cap = None
        return lst

    def merge(self, threads):
        threads = [t for t in threads if t]
        pos = [0] * len(threads)
        while True:
            best = None
            for k, t in enumerate(threads):
                if pos[k] < len(t):
                    fr = pos[k] / len(t)
                    if best is None or fr < best[0]:
                        best = (fr, k)
            if best is None:
                break
            k = best[1]
            self.add(*threads[k][pos[k]])
            pos[k] += 1

    def add(self, eng, fn, reads=(), writes=(), dma=False, grp="main"):
        if self._cap is not None:
            self._cap.append((eng, fn, tuple(reads), tuple(writes), dma, grp))
            return None
        idx = len(self.ops)
        deps = set()
        for t in reads:
            sp, s, e = t.reg
            for r in self.recs.get(sp, ()):
                if r[3] and r[0] < e and s < r[1]:
                    deps.add(r[2])
        for t in writes:
            sp, s, e = t.reg
            lst = self.recs.get(sp, [])
            keep = []
            for r in lst:
                if r[0] < e and s < r[1]:
                    deps.add(r[2])
                    if s <= r[0] and r[1] <= e:
                        continue
                keep.append(r)
            self.recs[sp] = keep
        tag = eng + ("_dma" if dma else "")
        for t in reads:
            sp, s, e = t.reg
            lst = self.recs.setdefault(sp, [])
            if not dma:
                for r in lst:
                    if (not r[3]) and r[0] == s and r[1] == e and r[4] == tag:
                        r[2] = idx
                        break
                else:
                    lst.append([s, e, idx, False, tag])
            else:
                lst.append([s, e, idx, False, tag])
        for t in writes:
            sp, s, e = t.reg
            self.recs.setdefault(sp, []).append([s, e, idx, True, tag])
        deps.discard(idx)
        self.ops.append(dict(eng=eng, fn=fn, deps=deps, dma=dma, grp=grp))
        return idx

    def emit(self, nc, block, eng_sems, dma_sems):
        ops = self.ops
        n = len(ops)
        dependents = [False] * n
        for i, o in enumerate(ops):
            for d in o["deps"]:
                od = ops[d]
                if od["eng"] == "pe" and o["eng"] == "pe" and not od["dma"] and not o["dma"]:
                    continue
                dependents[d] = True
        sig = [None] * n
        prev_dma = [None] * n
        cnt = {}
        dma_i = {g: 0 for g in dma_sems}
        uses = {g: [0] * len(v) for g, v in dma_sems.items()}
        for i, o in enumerate(ops):
            if o["fn"] is None:
                continue
            if o["dma"]:
                g = o["grp"]
                s = dma_i[g] % len(dma_sems[g])
                dma_i[g] += 1
                uses[g][s] += 1
                sig[i] = (dma_sems[g][s], 16 * uses[g][s])
                if uses[g][s] > 1:
                    prev_dma[i] = (dma_sems[g][s], 16 * (uses[g][s] - 1))
            elif dependents[i]:
                e = o["eng"]
                c = cnt.get(e, 0)
                cnt[e] = c + 1
                sig[i] = (eng_sems[e][c // self.LIMIT], c % self.LIMIT + 1)
        self.counts = cnt

        def make(engname):
            def body(eng):
                waited = {}
                for i, o in enumerate(ops):
                    if o["eng"] != engname:
                        continue
                    waits = []
                    for d in sorted(o["deps"]):
                        if sig[d] is None:
                            continue
                        od = ops[d]
                        if engname == "pe" and od["eng"] == "pe" and not od["dma"] and not o["dma"]:
                            continue
                        waits.append(sig[d])
                    if prev_dma[i] is not None:
                        waits.append(prev_dma[i])
                    for (sm, v) in waits:
                        if waited.get(sm.num, 0) < v:
                            eng.wait_ge(sm, v)
                            waited[sm.num] = v
                    if o["fn"] is not None:
                        ins = o["fn"](eng)
                        if sig[i] is not None:
                            ins.then_inc(sig[i][0], 16 if o["dma"] else 1)
            return body

        block.tensor(make("pe"))
        block.scalar(make("act"))
        block.vector(make("dve"))
        block.gpsimd(make("pool"))
        block.sync(make("sp"))


def build_nc():
    nc = bass.Bass("TRN2", target_bir_lowering=False)
    P = Prog()

    def din(name, shape, dt=F32):
        return nc.dram_tensor(name, list(shape), dt, kind="ExternalInput").ap()

    x_d = din("x", [S, D])
    win_d = din("w_in", [D, NIN])
    wpool_d = din("w_pool", [4, 128, 128])
    wbra_d = din("w_br_a", [512, D])
    wbrb_d = din("w_br_b", [512, D])
    wout_d = din("w_out", [D, D])
    weg_h = nc.dram_tensor("w_e_gate", [8192, 2048], F32, kind="ExternalInput")
    weu_h = nc.dram_tensor("w_e_up", [8192, 2048], F32, kind="ExternalInput")
    wed_h = nc.dram_tensor("w_e_down", [8192, 2048], F32, kind="ExternalInput")
    zx_d = din("zx", [128, 16384], BF16)
    wb_h = [nc.dram_tensor(f"wb{m}", [4096, 4096], BF16) for m in range(3)]
    xs_h = nc.dram_tensor("xs_s", [NROWS, D], BF16)
    ys_h = nc.dram_tensor("ys_s", [NROWS, D], F32)
    cpk_d = din("cpk", [128, NCPK])
    cbf_d = din("cbf", [128, NCBF], BF16)
    y_d = nc.dram_tensor("y", [S, D], F32, kind="ExternalOutput").ap()
    x1_d = nc.dram_tensor("x1s", [S, D], F32).ap()

    def dreg(name, ap, lo, hi):
        return Tl(ap, ("dram:" + name, lo, hi))

    ARENA_BYTES = 212480
    with (
        nc.sbuf_tensor("arena", [128, ARENA_BYTES // 4], F32) as arena,
        nc.psum_tensor("ps", [128, 8, 512], F32) as ps,
    ):
        def sview(off, shape, dt):
            esz = 4 if dt in (F32, I32) else 2
            nel = 1
            for s_ in shape[1:]:
                nel *= s_
            nb = nel * esz
            assert off % 4 == 0 and nb % 4 == 0, (off, shape)
            assert off + nb <= ARENA_BYTES, (off, nb)
            ap = arena[:, off // 4:(off + nb) // 4]
            if dt != F32:
                ap = ap.bitcast(dt)
            if len(shape) == 3:
                ap = ap.rearrange("p (a b) -> p a b", a=shape[1])
            return Tl(ap, ("sb", off, off + nb))

        class Bump:
            def __init__(self, lo, hi):
                self.o = lo
                self.hi = hi

            def __call__(self, shape, dt):
                esz = 4 if dt in (F32, I32) else 2
                nel = 1
                for s_ in shape[1:]:
                    nel *= s_
                nb = (nel * esz + 3) // 4 * 4
                t = sview(self.o, shape, dt)
                self.o += nb
                assert self.o <= self.hi, (self.o, self.hi)
                return t

        psi = [0]
        bpool = [list(range(8))]

        def bank():
            pool = bpool[0]
            b = pool[psi[0] % len(pool)]
            psi[0] += 1
            return Tl(ps[:, b, :], ("ps", b * 2048, (b + 1) * 2048))

        def cap(f, banks):
            old = bpool[0]
            bpool[0] = banks
            lst = P.capture(f)
            bpool[0] = old
            return lst

        def bf_view(bk, a, b):
            return bk.v(bk.ap.bitcast(BF16)[:, 0:a * b].rearrange("p (a b) -> p a b", a=a))

        def f_view(bk, a, b):
            return bk.v(bk.ap[:, 0:a * b].rearrange("p (a b) -> p a b", a=a))

        def mm(out, lhsT, rhs, start, stop):
            P.add("pe", lambda e, o=out.ap, l=lhsT.ap, r=rhs.ap, st=start, sp=stop:
                  e.matmul(o, l, r, start=st, stop=sp), reads=[lhsT, rhs], writes=[out])

        def tr(out, in_, ident):
            P.add("pe", lambda e, o=out.ap, i=in_.ap, d=ident.ap: e.transpose(o, i, d),
                  reads=[in_, ident], writes=[out])

        def act(out, in_, func, scale=1.0, bias=None):
            rd = [in_] + ([bias] if bias is not None else [])
            if bias is None:
                P.add("act", lambda e, o=out.ap, i=in_.ap, f=func, s=scale:
                      e.activation(o, i, f, scale=s), reads=rd, writes=[out])
            else:
                P.add("act", lambda e, o=out.ap, i=in_.ap, f=func, s=scale, b=bias.ap:
                      e.activation(o, i, f, bias=b, scale=s), reads=rd, writes=[out])

        def tt(out, a, b, op, eng="dve"):
            P.add(eng, lambda e, o=out.ap, x=a.ap, y=b.ap, p=op: e.tensor_tensor(o, x, y, p),
                  reads=[a, b], writes=[out])

        def ts(out, a, s1, s2, op0, op1=None, eng="dve"):
            rd = [a] + [s for s in (s1, s2) if isinstance(s, Tl)]
            v1 = s1.ap if isinstance(s1, Tl) else s1
            v2 = s2.ap if isinstance(s2, Tl) else s2
            if op1 is None:
                P.add(eng, lambda e, o=out.ap, x=a.ap, p=op0: e.tensor_scalar(o, x, v1, None, p),
                      reads=rd, writes=[out])
            else:
                P.add(eng, lambda e, o=out.ap, x=a.ap, p=op0, q=op1: e.tensor_scalar(o, x, v1, v2, p, q),
                      reads=rd, writes=[out])

        def stt(out, a, sc, b, op0, op1):
            rd = [a, b] + ([sc] if isinstance(sc, Tl) else [])
            sv = sc.ap if isinstance(sc, Tl) else sc
            P.add("dve", lambda e, o=out.ap, x=a.ap, y=b.ap, p=op0, q=op1:
                  e.scalar_tensor_tensor(o, x, sv, y, p, q), reads=rd, writes=[out])

        def red(out, in_, op):
            P.add("dve", lambda e, o=out.ap, i=in_.ap, p=op: e.tensor_reduce(o, i, AX.X, p),
                  reads=[in_], writes=[out])

        def recip(out, in_):
            P.add("dve", lambda e, o=out.ap, i=in_.ap: e.reciprocal(o, i), reads=[in_], writes=[out])

        def cp(out, in_, eng="dve"):
            if eng == "act":
                P.add("act", lambda e, o=out.ap, i=in_.ap: e.copy(o, i), reads=[in_], writes=[out])
            else:
                P.add(eng, lambda e, o=out.ap, i=in_.ap: e.tensor_copy(o, i), reads=[in_], writes=[out])

        def memset(out, val, eng="pool"):
            P.add(eng, lambda e, o=out.ap, v=val: e.memset(o, v), writes=[out])

        def dma(out, in_):
            return P.add("sp", lambda e, o=out.ap, i=in_.ap: e.dma_start(out=o, in_=i),
                         reads=[in_], writes=[out], dma=True)

        def idma(fn, reads, writes, grp="main"):
            return P.add("pool", fn, reads=reads, writes=writes, dma=True, grp=grp)

        def bc(t, shape, axis):
            return t.v(t.ap.unsqueeze(axis).to_broadcast(shape))

        CB = Bump(0, 11328)
        CPK = CB([128, NCPK], F32)
        CBF = CB([128, NCBF], BF16)
        X0 = 11328
        W0 = X0 + 65536
        S0 = W0 + 107648
        assert S0 == 184512

        def cslice(c0, n):
            return CPK[:, c0:c0 + n]

        GMIX = cslice(C_GMIX, 8)
        GFFN = cslice(C_GFFN, 8)
        PSC = cslice(C_PSC, 4)
        GHEAD = cslice(C_GHEAD, 4)
        CQ = CPK.v(CPK.ap[:, C_CQ:C_CQ + 16].rearrange("p (h j) -> p h j", h=4))
        CK = CPK.v(CPK.ap[:, C_CK:C_CK + 16].rearrange("p (h j) -> p h j", h=4))
        BIF = cslice(C_BIF, 8)
        BR = cslice(C_BR, 36)
        EPS_T = cslice(C_EPS, 1)
        ONE_T = cslice(C_ONE, 1)
        WR = CPK.v(CPK.ap[:, C_WR:C_WR + 288].rearrange("p (k n) -> p k n", k=8))
        IDF = cslice(C_IDF, 128)
        TRIF = cslice(C_TRI, 128)
        ONESF = cslice(C_ONES, 128)
        GFIN = cslice(C_GFIN, 1024)
        IDB = CBF[:, B_ID:B_ID + 128]
        PA = CBF.v(CBF.ap[:, B_PA:B_PA + 512].rearrange("p (g t) -> p g t", g=4))
        PB = CBF.v(CBF.ap[:, B_PB:B_PB + 512].rearrange("p (g t) -> p g t", g=4))
        PA0 = CBF.v(CBF.ap[:, B_PA0:B_PA0 + 512].rearrange("p (g t) -> p g t", g=4))
        TRIS = CBF[:, B_TRIS:B_TRIS + 128]
        ONESB = CBF[:, B_ONESB:B_ONESB + 128]
        GFFN2 = cslice(C_GFFN2, 8)
        PIDX2 = cslice(C_PIDX2, 1)
        THR16 = cslice(C_THR16, 16)
        THR48 = cslice(C_THR48, 48)

        dma(CPK, dreg("cpk", cpk_d, 0, 1))
        dma(CBF, dreg("cbf", cbf_d, 0, 1))

        WB = Bump(W0, S0)
        WIN = WB([128, 8, NIN], BF16)
        WPOOL = WB([128, 4, 128], BF16)
        WBRA = WB([128, 4, D], BF16)
        WBRB = WB([128, 4, D], BF16)
        WOUT = WB([128, 8, D], BF16)

        XB_ = Bump(X0, X0 + 65536)
        SBX = Bump(S0, ARENA_BYTES)
        XT = [XB_([128, D], F32) for _ in range(2)] + [SBX([128, D], F32), SBX([128, D], F32)]
        XS = XB_([128, D], BF16)
        HT_ = XB_([128, 8, 128], BF16)
        U = [XB_([128, 512], BF16) for _ in range(2)]
        VAUG = [XB_([128, 4, 130], BF16), SBX([128, 4, 130], BF16)]
        QC = XB_([128, 4, 132], F32)
        KC = XB_([128, 4, 132], F32)
        QACC = XB_([128, 4, 128], F32)
        KACC = XB_([128, 4, 128], F32)
        QT = [XB_([128, 4, 128], BF16), SBX([128, 4, 128], BF16)]
        KT = [XB_([128, 4, 128], BF16), SBX([128, 4, 128], BF16)]
        KP = [XB_([128, 4, 128], BF16), SBX([128, 4, 128], BF16)]
        SDT = XB_([128, 4, 128], BF16)
        C32 = XB_([128, 4, 129], F32)
        CBFS = XB_([128, 4, 130], BF16)
        SIGO = [XB_([128, 512], F32), SBX([128, 512], F32)]
        SGA = XB_([128, D], BF16)
        SGB = [XB_([128, D], BF16), SBX([128, D], BF16)]
        DT_ = XB_([128, 4, 128], BF16)
        YPT = XB_([128, 4, 128], BF16)
        HN = XB_([128, 4, 128], F32)
        HNO = XB_([128, 512], BF16)
        HMT = XB_([128, 4, 128], BF16)
        SMS = [XB_([128, 128], F32), SBX([128, 128], F32)]
        M1 = [XB_([128, D], F32), SBX([128, D], F32)]
        X1T = [XB_([128, D], F32), SBX([128, D], F32)]
        MRG = XB_([128, D], BF16)
        MT_ = XB_([128, 8, 128], BF16)
        TMPQK = sview(QACC.reg[1], [128, D], F32)
        assert KACC.reg[1] == QACC.reg[2]
        STG_N = 8
        STG = [sview(X0 + 65536 - (j + 1) * 4616, [128, 1154], F32) for j in range(STG_N)]

        class SmallSet:
            pass

        SMV = []
        for SM in SMS:
            smi = [0]

            def sm(n, SM=SM, smi=smi):
                t = SM[:, smi[0]:smi[0] + n]
                smi[0] += n
                assert smi[0] <= 128
                return t

            v_ = SmallSet()
            v_.SS, v_.SD_, v_.RSTD = sm(1), sm(1), sm(1)
            v_.GIF, v_.E1, v_.L1, v_.TMP4, v_.RR, v_.EB, v_.EBL = sm(8), sm(4), sm(4), sm(4), sm(4), sm(4), sm(4)
            (v_.NQV, v_.DEN, v_.RDEN, v_.FAC, v_.F2, v_.VH, v_.SDH, v_.RSH, v_.SC) = (sm(4) for _ in range(9))
            v_.ST6 = SM.v(sm(24).ap.rearrange("p (h s) -> p h s", h=4))
            v_.MV = SM.v(sm(8).ap.rearrange("p (h s) -> p h s", h=4))
            SMV.append(v_)

        cast_engs = ["act", "dve"]
        pcs = [0]

        def load_cast(dst, src_ap, src_reg, ncols):
            j = pcs[0]
            pcs[0] += 1
            st = STG[j % STG_N]
            stv = st[:, 0:ncols]
            dma(stv, dreg(*src_reg).v(src_ap))
            cp(dst, stv, eng=cast_engs[j % 2])

        for k in range(8):
            for c in range(4):
                load_cast(WIN[:, k, c * 1154:(c + 1) * 1154],
                          win_d[k * 128:(k + 1) * 128, c * 1154:(c + 1) * 1154],
                          ("w_in", win_d, 0, 1), 1154)
        st = STG[pcs[0] % STG_N]
        pcs[0] += 1
        stv = st.v(st.ap[:, 0:512].rearrange("p (g d) -> p g d", g=4))
        dma(stv, dreg("w_pool", wpool_d.rearrange("g c d -> c g d"), 0, 1))
        cp(WPOOL, stv, eng="dve")
        for g in range(4):
            load_cast(WBRA[:, g, :], wbra_d[g * 128:(g + 1) * 128, :], ("w_br_a", wbra_d, 0, 1), 1024)
        for g in range(4):
            load_cast(WBRB[:, g, :], wbrb_d[g * 128:(g + 1) * 128, :], ("w_br_b", wbrb_d, 0, 1), 1024)
        for k in range(8):
            load_cast(WOUT[:, k, :], wout_d[k * 128:(k + 1) * 128, :], ("w_out", wout_d, 0, 1), 1024)

        xs_flat = xs_h.ap().rearrange("(a b) f -> a (b f)", b=16)

        def zero_fill(c):
            dma(Tl(xs_flat[c * 128:(c + 1) * 128, :], ("dram:xs", 0, 64)), dreg("zx", zx_d, 0, 1))
        for b_ in range(2):
            memset(VAUG[b_], 1.0)
        memset(QC, 0.0)
        memset(KC, 0.0)
        memset(C32, 0.0)
        memset(CBFS, 0.0)

        LNS = math.log(DH ** -0.5)

        def xrow(i):
            return dreg("x", x_d[i * 128:(i + 1) * 128, :], i, i + 1)

        def x1row(i):
            return dreg("x1s", x1_d[i * 128:(i + 1) * 128, :], i, i + 1)

        def yrow(i):
            return dreg("y", y_d[i * 128:(i + 1) * 128, :], i, i + 1)

        def proj(col0, ncols):
            pb = bank()
            o = pb[:, 0:ncols]
            for k in range(8):
                mm(o, HT_[:, k, :], WIN[:, k, col0:col0 + ncols], k == 0, k == 7)
            return o

        def A1(i):
            b = i % 2
            xt = XT[i % 4]
            V = SMV[b]
            act(TMPQK, xt, AF.Square)
            red(V.SS, TMPQK, ALU.add)
            act(V.SD_, V.SS, AF.Sqrt, scale=1.0 / D, bias=EPS_T)
            recip(V.RSTD, V.SD_)
            ts(XS, xt, V.RSTD, None, ALU.mult)
            pt = bank()
            ptb = bf_view(pt, 8, 128)
            for k in range(8):
                tr(ptb[:, k, :], XS[:, k * 128:(k + 1) * 128], IDB)
            tt(HT_, ptb, bc(GMIX, [128, 8, 128], 2), ALU.mult)
            pu = proj(0, 512)
            cp(U[b], pu, eng="act")
            pv = proj(1536, 512)
            cp(VAUG[b][:, :, 0:128], pv.v(pv.ap.rearrange("p (h d) -> p h d", h=4)), eng="dve")
            po = proj(2048, 512)
            act(SIGO[b], po, AF.Sigmoid)
            pif = proj(2560, 8)
            tt(V.GIF, pif, BIF, ALU.add)
            act(V.E1, V.GIF[:, 4:8], AF.Exp, scale=-1.0)
            act(V.L1, V.E1, AF.Ln, bias=ONE_T)
            pgt = bank()
            mm(pgt[:, 0:4], TRIF, V.L1, True, True)
            mm(pgt[:, 4:8], ONESF, V.L1, True, True)
            stt(V.TMP4, V.GIF[:, 0:4], LNS, pgt[:, 0:4], ALU.add, ALU.add)
            act(V.RR, V.TMP4, AF.Exp)
            act(V.EB, pgt[:, 0:4], AF.Exp, scale=-1.0)
            act(V.EBL, pgt[:, 4:8], AF.Exp, scale=-1.0)

        def A2(i):
            b = i % 2
            ucur, uprev = U[b], U[(i + 1) % 2]
            for hf in range(2):
                pg_ = proj(2568 + hf * 512, 512)
                act(SGA[:, hf * 512:(hf + 1) * 512], pg_, AF.Sigmoid)
            for hf in range(2):
                pg_ = proj(3592 + hf * 512, 512)
                act(SGB[b][:, hf * 512:(hf + 1) * 512], pg_, AF.Sigmoid)
            pd = bank()
            pdv = f_view(pd, 4, 128)
            for g in range(4):
                mm(pdv[:, g, :], ucur[:, g * 128:(g + 1) * 128], (PA0 if i == 0 else PA)[:, g, :], True, i == 0)
                if i > 0:
                    mm(pdv[:, g, :], uprev[:, g * 128:(g + 1) * 128], PB[:, g, :], False, True)
            cp(DT_, pdv, eng="act")
            py = bank()
            pyv = f_view(py, 4, 128)
            for g in range(4):
                mm(pyv[:, g, :], WPOOL[:, g, :], DT_[:, g, :], True, True)
            tt(YPT, pyv, bc(PSC, [128, 4, 128], 2), ALU.mult)
            pa = [bank() for _ in range(2)]
            for hf in range(2):
                for g in range(4):
                    mm(pa[hf], YPT[:, g, :], WBRA[:, g, hf * 512:(hf + 1) * 512], g == 0, g == 3)
            for hf in range(2):
                sl = slice(hf * 512, (hf + 1) * 512)
                tt(M1[b][:, sl], pa[hf], SGA[:, sl], ALU.mult)

        def A3(i):
            b = i % 2
            V = SMV[b]
            for (col0, buf) in ((512, QC), (1024, KC)):
                pq = bank()
                pqv = f_view(pq, 4, 128)
                for h in range(4):
                    for k in range(8):
                        mm(pqv[:, h, :], WIN[:, k, col0 + h * 128:col0 + (h + 1) * 128], HT_[:, k, :],
                           k == 0, k == 7)
                cp(buf[:, :, 3:131], pqv, eng="act")
            TMPQ = TMPQK.v(TMPQK.ap[:, 0:512].rearrange("p (h t) -> p h t", h=4))
            TMPK = TMPQK.v(TMPQK.ap[:, 512:1024].rearrange("p (h t) -> p h t", h=4))
            for (buf, acc, cw, tmp, outT) in ((QC, QACC, CQ, TMPQ, QT[b]), (KC, KACC, CK, TMPK, KT[b])):
                for j in range(4):
                    for h in range(4):
                        ah = acc.chunk(h, 4)
                        if j == 0:
                            ts(ah, buf[:, h, 0:128], cw[:, h, 0:1], None, ALU.mult)
                        else:
                            stt(ah, buf[:, h, j:j + 128], cw[:, h, j:j + 1], ah, ALU.mult, ALU.add)
                act(outT, acc, AF.Silu)
                cp(buf[:, :, 0:3], buf[:, :, 128:131], eng="dve")
            pk = bank()
            pkb = bf_view(pk, 4, 128)
            for h in range(4):
                tr(pkb[:, h, :], KT[b][:, h, :], IDB)
            tt(KP[b], pkb, bc(V.RR, [128, 4, 128], 2), ALU.mult)

        def B1(i):
            b = i % 2
            V = SMV[b]
            psb = bank()
            psv = f_view(psb, 4, 128)
            for h in range(4):
                mm(psv[:, h, :], KT[b][:, h, :], QT[b][:, h, :], True, True)
            for h in range(4):
                stt(SDT[:, h, :], psv[:, h, :], V.RR[:, h:h + 1], TRIF, ALU.mult, ALU.mult)
            pn = [f_view(bank(), 2, 129) for _ in range(2)]
            for h in range(4):
                o = pn[h // 2][:, h % 2, :]
                mm(o, SDT[:, h, :], VAUG[b][:, h, 0:129], True, i == 0)
                if i > 0:
                    mm(o, QT[b][:, h, :], CBFS[:, h, 0:129], False, True)
            if i + 1 < NT:
                pc = [f_view(bank(), 2, 129) for _ in range(2)]
                for h in range(4):
                    mm(pc[h // 2][:, h % 2, :], KP[b][:, h, :], VAUG[b][:, h, 0:129], True, True)
            for p_ in range(2):
                tt(V.NQV[:, 2 * p_:2 * p_ + 2], pn[p_][:, :, 128], V.EB[:, 2 * p_:2 * p_ + 2], ALU.mult)
            ts(V.RDEN, V.NQV, -1.0, None, ALU.mult)
            tt(V.DEN, V.NQV, V.RDEN, ALU.max)
            ts(V.DEN, V.DEN, 1.0, None, ALU.max)
            recip(V.RDEN, V.DEN)
            tt(V.FAC, V.EB, V.RDEN, ALU.mult)
            for h in range(4):
                P.add("dve", lambda e, o=V.ST6.ap[:, h, :], i_=pn[h // 2].ap[:, h % 2, 0:128]: e.bn_stats(o, i_),
                      reads=[pn[h // 2]], writes=[V.ST6])
                P.add("dve", lambda e, o=V.MV.ap[:, h, :], i_=V.ST6.ap[:, h, :]: e.bn_aggr(o, i_),
                      reads=[V.ST6], writes=[V.MV])
            tt(V.F2, V.FAC, V.FAC, ALU.mult)
            tt(V.VH, V.F2, V.MV[:, :, 1], ALU.mult)
            act(V.SDH, V.VH, AF.Sqrt, bias=EPS_T)
            recip(V.RSH, V.SDH)
            tt(V.SC, V.FAC, V.RSH, ALU.mult)
            for h in range(4):
                ts(HN[:, h, :], pn[h // 2][:, h % 2, 0:128], V.MV[:, h, 0:1], V.SC[:, h:h + 1],
                   ALU.subtract, ALU.mult)
            tt(HNO, HN.v(HN.ap.rearrange("p h d -> p (h d)")), SIGO[b], ALU.mult)
            if i + 1 < NT:
                for p_ in range(2):
                    tt(C32[:, 2 * p_:2 * p_ + 2, :], C32[:, 2 * p_:2 * p_ + 2, :], pc[p_], ALU.add)
                    tt(C32[:, 2 * p_:2 * p_ + 2, :], C32[:, 2 * p_:2 * p_ + 2, :],
                       V.EBL.v(V.EBL.ap[:, 2 * p_:2 * p_ + 2].unsqueeze(2).to_broadcast([128, 2, 129])), ALU.mult)
                cp(CBFS[:, :, 0:129], C32, eng="act")

        def B2a(i):
            b = i % 2
            ph = bank()
            phb = bf_view(ph, 4, 128)
            for h in range(4):
                tr(phb[:, h, :], HNO[:, h * 128:(h + 1) * 128], IDB)
            tt(HMT, phb, bc(GHEAD, [128, 4, 128], 2), ALU.mult)
            pbb = [bank() for _ in range(2)]
            for hf in range(2):
                for h in range(4):
                    mm(pbb[hf], HMT[:, h, :], WBRB[:, h, hf * 512:(hf + 1) * 512], h == 0, h == 3)
            for hf in range(2):
                sl = slice(hf * 512, (hf + 1) * 512)
                tt(X1T[b][:, sl], pbb[hf], SGB[b][:, sl], ALU.mult)

        def B2b(i):
            b = i % 2
            xt = XT[i % 4]
            tt(MRG, M1[b], X1T[b], ALU.add)
            pm = bank()
            pmb = bf_view(pm, 8, 128)
            for k in range(8):
                tr(pmb[:, k, :], MRG[:, k * 128:(k + 1) * 128], IDB)
            cp(MT_, pmb, eng="act")
            pox = [bank() for _ in range(2)]
            for hf in range(2):
                for k in range(8):
                    mm(pox[hf], MT_[:, k, :], WOUT[:, k, hf * 512:(hf + 1) * 512], k == 0, k == 7)
            for hf in range(2):
                sl = slice(hf * 512, (hf + 1) * 512)
                tt(X1T[b][:, sl], pox[hf], xt[:, sl], ALU.add)
            dma(yrow(i) if STAGE == "mixer" else x1row(i), X1T[b])
            if i + 4 < NT:
                dma(XT[i % 4], xrow(i + 4))
            if STAGE != "mixer" and i % 2 == 1 and i // 2 < NROWS // 16 // 128:
                zero_fill(i // 2)

        dma(XT[0], xrow(0))
        dma(XT[1], xrow(1))
        dma(XT[2], xrow(2))
        dma(XT[3], xrow(3))
        WSRC = (weg_h, weu_h, wed_h)
        NCV = 32

        def conv_w(n_, gate=None):
            m_, c = divmod(n_, NCV)
            if m_ < 2:
                r0, r1 = c * 8192 // NCV, (c + 1) * 8192 // NCV
                dst_ap = wb_h[m_].ap().rearrange("r (h m) -> (r h) m", h=2)[r0:r1, :]
                src_ap = WSRC[m_][r0:r1, :]
            else:
                assert NCV == 32
                src_ap = WSRC[2].ap().rearrange("r (h m) -> (r h) m", h=2)[c * 512:(c + 1) * 512, :].rearrange(
                    "(k p) n -> p k n", p=128)
                dst_ap = wb_h[2].ap().rearrange("(e p) (k n) -> e p k n", p=128, k=4)[c]
            rd = [Tl(None, ("fake:cv", n_ - 4, n_ - 3))] if n_ >= 4 else []
            if gate is not None:
                rd.append(gate)
            idma(lambda e, o=dst_ap, src=src_ap: e.dma_start(out=o, in_=src),
                 reads=rd, writes=[Tl(None, ("dram:wb%d" % m_, c, c + 1)), Tl(None, ("fake:cv", n_, n_ + 1))],
                 grp="cv")
        A1(0)
        if STAGE != "mixer":
            for n_ in range(3 * NCV):
                conv_w(n_, gate=(XS if n_ == 0 else None))
        P.merge([cap(lambda: A3(0), [0, 1, 2, 3]), cap(lambda: A2(0), [4, 5, 6, 7])])
        for i in range(NT):
            nx = i + 1 < NT
            th = [cap(lambda: B1(i), [0, 1, 2, 3, 4])]
            if nx:
                th.append(cap(lambda: A1(i + 1), [5, 6, 7]))
            P.merge(th)
            th = [cap(lambda: (B2a(i), B2b(i)), [0, 1, 2])]
            if nx:
                th.append(cap(lambda: A3(i + 1), [3, 4]))
                th.append(cap(lambda: A2(i + 1), [5, 6, 7]))
            P.merge(th)

        out_dmas = []
        if STAGE != "mixer":
            def xres(i, hf=None):
                if hf is None:
                    return sview(X0 + i * 4096, [128, D], F32)
                return sview(X0 + i * 4096 + hf * 2048, [128, 512], F32)

            MB = Bump(W0, S0)
            XSB = MB([128, NT, D], BF16)
            WG = [MB([128, 8, 512], BF16) for _ in range(2)]
            WU = [MB([128, 8, 512], BF16) for _ in range(2)]
            WD = [MB([128, 4, D], BF16) for _ in range(2)]
            ESTG = [MB([128, 2048], F32) for _ in range(3)]

            def sv(off, shape, dt):
                return sview(S0 + off, shape, dt)

            DESTI = sv(0, [128, 32], I32)
            GK = sv(128, [128, 32], F32)
            IDXI = sv(256, [128, NBLK], I32)
            E0 = ESTG[0].reg[1]
            RSA = [sv(640, [128, 160], F32), sv(1280, [128, 160], F32),
                   sview(E0 + 16384, [128, 160], F32), sview(E0 + 17024, [128, 160], F32)]
            RS = sv(1920, [128, 416], F32)
            OHK = sv(3584, [128, NT * 2 * 32], F32)
            OHSB = sv(7680, [128, NT, 32], BF16)
            RANK = sv(8704, [128, NT, 32], F32)
            XS32 = [sv(10752, [128, D], F32), sv(18944, [128, D], F32),
                    sview(E0, [128, D], F32), sview(E0 + 8192, [128, D], F32)]
            HF32 = [sv(14848, [128, 8, 128], F32), sv(23040, [128, 8, 128], F32),
                    sview(E0 + 4096, [128, 8, 128], F32), sview(E0 + 12288, [128, 8, 128], F32)]
            TMPD = sv(10752, [128, NT * 2 * 32], F32)
            TMPB = sv(14848, [128, NBLK * 32], F32)
            NB16 = sv(20992, [128, 32 * 16], F32)
            XB = sv(3584, [128, 2, D], BF16)
            XTB = sv(7680, [128, 8, 256], BF16)
            HTB = [sv(11776, [128, 4, 256], BF16), sv(13824, [128, 4, 256], BF16)]
            SGb = [sv(15872, [128, 256], F32), sv(16896, [128, 256], F32)]
            YB = sv(17920, [128, 2, D], F32)
            YG = [sv(3584 + n_ * 4096, [128, D], F32) for n_ in range(3)]
            OUTT = [sv(15872, [128, D], F32), sv(19968, [128, D], F32)]
            assert S0 + 27136 <= ARENA_BYTES
            rsi = [0]

            def rs(n):
                t = RS[:, rsi[0]:rsi[0] + n]
                rsi[0] += n
                assert rsi[0] <= 416
                return t

            class RT:
                pass

            RTS = []
            for RSx in RSA:
                c_ = [0]

                def ra(n, RSx=RSx, c_=c_):
                    t = RSx[:, c_[0]:c_[0] + n]
                    c_[0] += n
                    assert c_[0] <= 160
                    return t

                r_ = RT()
                r_.SS2, r_.SD2, r_.RSTD2 = ra(1), ra(1), ra(1)
                r_.LG = ra(36)
                r_.GMAX, r_.OHG, r_.GSH, r_.GE, r_.GSUM, r_.PGV = ra(1), ra(4), ra(4), ra(4), ra(1), ra(1)
                r_.T32 = RSx.v(ra(32).ap.rearrange("p (g j) -> p g j", g=4))
                r_.ESEL, r_.M1_, r_.OH1, r_.MSK, r_.M2_, r_.OH2 = ra(8), ra(1), ra(8), ra(8), ra(1), ra(8)
                r_.DD, r_.E2, r_.DEN2, r_.W1, r_.W2 = (ra(1) for _ in range(5))
                RTS.append(r_)
            SS2, SD2, RSTD2 = RTS[0].SS2, RTS[0].SD2, RTS[0].RSTD2
            CUM, NBLKT, PADA, CSA, CSB_, POFF = (rs(32) for _ in range(6))
            BEF, EMP, IDXF, IDXF1 = (rs(NBLK) for _ in range(4))
            DESTF = rs(32)
            ohk4 = OHK.v(OHK.ap.rearrange("p (i k e) -> p i k e", i=NT, k=2))

            def sub(t, ap, off, nb):
                sp_, s0_, _ = t.reg
                return Tl(ap, (sp_, s0_ + off, s0_ + off + nb))

            for i in range(NT):
                dma(xres(i), x1row(i))

            def route(i, th):
                R = RTS[th]
                xs32, hf32 = XS32[th], HF32[th]
                sqs = hf32.v(hf32.ap.rearrange("p k t -> p (k t)"))
                xi = xres(i)
                act(sqs, xi, AF.Square)
                red(R.SS2, sqs, ALU.add)
                act(R.SD2, R.SS2, AF.Sqrt, scale=1.0 / D, bias=EPS_T)
                recip(R.RSTD2, R.SD2)
                ts(xs32, xi, R.RSTD2, None, ALU.mult)
                cp(sub(XSB, XSB.ap[:, i, :], i * 2048, 2048), xs32, eng="act")
                pp = [bank() for _ in range(2)]
                for k in range(8):
                    tr(pp[k // 4][:, (k % 4) * 128:(k % 4 + 1) * 128], xs32[:, k * 128:(k + 1) * 128], IDF)
                for hf in range(2):
                    tt(hf32[:, 4 * hf:4 * hf + 4, :], f_view(pp[hf], 4, 128),
                       GFFN.v(GFFN.ap[:, 4 * hf:4 * hf + 4].unsqueeze(2).to_broadcast([128, 4, 128])), ALU.mult)
                pr = bank()
                for k in range(8):
                    mm(pr[:, 0:36], hf32[:, k, :], WR[:, k, :], k == 0, k == 7)
                lg4 = LG4B[(i // 4) % 2]
                tt(sub(lg4, lg4.ap[:, th, :], th * 144, 144), pr[:, 0:36], BR, ALU.add)

            BT = sview(E0 + 17664, [128, 768], F32)
            bti = [0]

            def bt(n, shape=None):
                t = BT[:, bti[0]:bti[0] + n]
                bti[0] += n
                assert bti[0] <= 768
                if shape is not None:
                    t = t.v(t.ap.rearrange("p (a b) -> p a b", a=shape[0]))
                return t

            LG4B = [bt(144, (4, 36)), bt(144, (4, 36))]
            GMAX4, GSUM4, PGV4, M14, M24, DD4, E24, DEN4, W14, W24 = (bt(4) for _ in range(10))
            OHG4, GSH4, GE4 = bt(16, (4, 4)), bt(16, (4, 4)), bt(16, (4, 4))
            T324 = bt(128)
            ESEL4, OH14, MSK4, OH24 = (bt(32, (4, 8)) for _ in range(4))

            def b3(t, shape):
                return t.v(t.ap.unsqueeze(2).to_broadcast(shape))

            def route_small(g0):
                LG4 = LG4B[(g0 // 4) % 2]
                gl = LG4[:, :, 0:4]
                el = LG4.v(LG4.ap[:, :, 4:36].rearrange("p t (g j) -> p t g j", g=4))
                red(GMAX4, gl, ALU.max)
                tt(OHG4, gl, b3(GMAX4, [128, 4, 4]), ALU.is_equal)
                tt(GSH4, gl, b3(GMAX4, [128, 4, 4]), ALU.subtract)
                act(GE4, GSH4, AF.Exp)
                red(GSUM4, GE4, ALU.add)
                recip(PGV4, GSUM4)
                t4 = T324.v(T324.ap.rearrange("p (t g j) -> p t g j", t=4, g=4))
                tt(t4, el, OHG4.v(OHG4.ap.unsqueeze(3).to_broadcast([128, 4, 4, 8])), ALU.mult)
                red(ESEL4, T324.v(T324.ap.rearrange("p (t g j) -> p t j g", t=4, g=4)), ALU.add)
                red(M14, ESEL4, ALU.max)
                tt(OH14, ESEL4, b3(M14, [128, 4, 8]), ALU.is_equal)
                stt(MSK4, OH14, -1e30, ESEL4, ALU.mult, ALU.add)
                red(M24, MSK4, ALU.max)
                tt(OH24, MSK4, b3(M24, [128, 4, 8]), ALU.is_equal)
                tt(DD4, M24, M14, ALU.subtract)
                act(E24, DD4, AF.Exp)
                ts(DEN4, E24, 1.0, None, ALU.add)
                recip(W14, DEN4)
                tt(W24, E24, W14, ALU.mult)
                gk4 = GK.v(GK.ap[:, 2 * g0:2 * g0 + 8].rearrange("p (t k) -> p t k", k=2))
                tt(gk4[:, :, 0], PGV4, W14, ALU.mult)
                tt(gk4[:, :, 1], PGV4, W24, ALU.mult)
                ohg_b = OHG4.v(OHG4.ap.unsqueeze(3).to_broadcast([128, 4, 4, 8]))
                for k_, oh in ((0, OH14), (1, OH24)):
                    dst = OHK.v(ohk4.ap[:, g0:g0 + 4, k_, :].rearrange("p t (g j) -> p t g j", g=4))
                    tt(dst, ohg_b, oh.v(oh.ap.unsqueeze(2).to_broadcast([128, 4, 4, 8])), ALU.mult)
                tt(OHSB[:, g0:g0 + 4, :], ohk4[:, g0:g0 + 4, 0, :], ohk4[:, g0:g0 + 4, 1, :], ALU.add)

            for i in range(0, NT, 4):
                th = [cap(lambda: route(i + t_, t_), [2 * t_, 2 * t_ + 1]) for t_ in range(4)]
                if i >= 4:
                    th.append(cap(lambda: route_small(i - 4), [0]))
                P.merge(th)
            route_small(NT - 4)

            prk = bank()
            pct = bank()
            for i in range(NT):
                o = prk[:, i * 32:(i + 1) * 32]
                mm(o, TRIS, OHSB[:, i, :], True, i == 0)
                for i2 in range(i):
                    mm(o, ONESB, OHSB[:, i2, :], False, i2 == i - 1)
            for i in range(NT):
                mm(pct[:, 0:32], ONESB, OHSB[:, i, :], i == 0, i == NT - 1)
            cp(CUM, pct[:, 0:32])
            nb16 = NB16.v(NB16.ap.rearrange("p (e m) -> p e m", m=16))
            tt(nb16, CUM.v(CUM.ap.unsqueeze(2).to_broadcast([128, 32, 16])),
               THR16.v(THR16.ap.unsqueeze(1).to_broadcast([128, 32, 16])), ALU.is_gt)
            red(NBLKT, nb16, ALU.add)
            ts(PADA, NBLKT, 256.0, None, ALU.mult)
            cp(CSA, PADA)
            cur, nxt = CSA, CSB_
            for sh in (1, 2, 4, 8, 16):
                cp(nxt[:, 0:sh], cur[:, 0:sh])
                tt(nxt[:, sh:32], cur[:, sh:32], cur[:, 0:32 - sh], ALU.add)
                cur, nxt = nxt, cur
            PEND = cur
            tt(POFF, PEND, PADA, ALU.subtract)
            tt(RANK, prk.v(prk.ap.rearrange("p (i e) -> p i e", e=32)),
               POFF.v(POFF.ap.unsqueeze(1).to_broadcast([128, NT, 32])), ALU.add)
            tmpd4 = TMPD.v(TMPD.ap.rearrange("p (i k e) -> p i k e", i=NT, k=2))
            tt(tmpd4, ohk4, RANK.v(RANK.ap.unsqueeze(2).to_broadcast([128, NT, 2, 32])), ALU.mult)
            red(DESTF, TMPD.v(TMPD.ap.rearrange("p (n e) -> p n e", e=32)), ALU.add)
            cp(DESTI, DESTF)
            tmpb3 = TMPB.v(TMPB.ap.rearrange("p (j e) -> p j e", e=32))
            tt(tmpb3, PEND.v(PEND.ap.unsqueeze(1).to_broadcast([128, NBLK, 32])),
               THR48.v(THR48.ap.unsqueeze(2).to_broadcast([128, NBLK, 32])), ALU.is_le)
            red(BEF, tmpb3, ALU.add)
            ts(BEF, BEF, 31.0, None, ALU.min)
            ts(EMP, THR48, PEND[:, 31:32], None, ALU.is_ge)
            ts(IDXF, BEF, 256.0, PIDX2, ALU.mult, ALU.add)
            ts(IDXF, IDXF, 0.5, None, ALU.mult)
            stt(IDXF, EMP, float(1 << 21), IDXF, ALU.mult, ALU.add)
            cp(IDXI, IDXF)

            IOA = bass.IndirectOffsetOnAxis
            regcache = {}

            def breg(e, v):
                if v not in regcache:
                    regcache[v] = e.to_reg(v)
                return regcache[v]


            XSB_OFF = XSB.reg[1]
            XB2 = [XB, sview(XSB_OFF + 24576, [128, 2, D], BF16)]
            XTB2 = [XTB, sview(XSB_OFF + 28672, [128, 8, 256], BF16)]

            def gather_w(j, m_):
                dT = (WG, WU, WD)[m_][j % 2]
                dflat = dT.v(dT.ap.rearrange("p k n -> p (k n)"))
                idma(lambda e, o=dflat.ap, src=wb_h[m_][:, :], idx=IDXI.ap[:, j:j + 1]:
                     e.indirect_dma_start(out=o, out_offset=None, in_=src, in_offset=IOA(ap=idx, axis=0),
                                          bounds_check=breg(e, 4095), oob_is_err=False),
                     reads=[IDXI, Tl(None, ("dram:wb%d" % m_, 0, 64))], writes=[dflat])

            def load_x(j):
                dma(XB2[j % 2], Tl(xs_h.ap()[j * 256:(j + 1) * 256, :].rearrange("(r p) f -> p r f", p=128),
                                   ("dram:xs", 0, 64)))

            def blk_B1(j):
                xb = XB2[j % 2]
                xtb = XTB2[j % 2]
                for r in range(2):
                    pt = bank()
                    ptb = bf_view(pt, 8, 128)
                    xr_ = xb.v(xb.ap[:, r, :].rearrange("p (c k) -> p k c", k=8))
                    for k in range(8):
                        tr(ptb[:, k, :], xr_[:, k, :], IDB)
                    tt(xtb[:, :, r * 128:(r + 1) * 128], ptb, bc(GFFN2, [128, 8, 128], 2), ALU.mult)

            sgi = [0]

            def blk_B2(j, hcs):
                b = j % 2
                htb = HTB[j % 2]
                xtb = XTB2[j % 2]
                for hc in hcs:
                    pg = bank()
                    pu = bank()
                    for k in range(8):
                        mm(pg[:, 0:256], WG[b][:, k, hc * 128:(hc + 1) * 128], xtb[:, k, :], k == 0, k == 7)
                    for k in range(8):
                        mm(pu[:, 0:256], WU[b][:, k, hc * 128:(hc + 1) * 128], xtb[:, k, :], k == 0, k == 7)
                    sg = SGb[sgi[0] % 2]
                    sgi[0] += 1
                    act(sg, pg[:, 0:256], AF.Silu)
                    tt(htb[:, hc, :], sg, pu[:, 0:256], ALU.mult)

            def blk_B3(j):
                b = j % 2
                htb = HTB[j % 2]
                for r in range(2):
                    for hf in range(2):
                        py_ = bank()
                        for hc in range(4):
                            mm(py_, htb[:, hc, r * 128:(r + 1) * 128], WD[b][:, hc, hf * 512:(hf + 1) * 512],
                               hc == 0, hc == 3)
                        cp(YB[:, r, hf * 512:(hf + 1) * 512], py_, eng=("act" if (r + hf) % 2 == 0 else "dve"))
                dma(Tl(ys_h.ap()[j * 256:(j + 1) * 256, :].rearrange("(r p) f -> p r f", p=128),
                       ("dram:ys", j, j + 1)), YB)

            nblk_run = NBLK
            for m_ in range(3):
                gather_w(0, m_)
            for i in range(NT):
                for k_ in range(2):
                    n_ = 2 * i + k_
                    idma(lambda e, o=xs_h[:, :], idx=DESTI.ap[:, n_:n_ + 1], src=XSB.ap[:, i, :]:
                         e.indirect_dma_start(out=o, out_offset=IOA(ap=idx, axis=0), in_=src, in_offset=None,
                                              bounds_check=breg(e, NROWS - 1), oob_is_err=False),
                         reads=[XSB, DESTI], writes=[Tl(None, ("dram:xs", n_, n_ + 1))])

            load_x(0)
            blk_B1(0)
            load_x(1)
            for j in range(nblk_run):
                nx = j + 1 < nblk_run
                if nx:
                    for m_ in range(3):
                        gather_w(j + 1, m_)

                def t_main(j=j):
                    blk_B2(j, (0, 1, 2, 3))
                    blk_B3(j)

                def t_next(j=j):
                    blk_B1(j + 1)
                    if j + 2 < nblk_run:
                        load_x(j + 2)

                th = [cap(t_main, [0, 1, 2, 3, 4, 5])]
                if nx:
                    th.append(cap(t_next, [6, 7]))
                P.merge(th)

            YGR = YG + [sview(E0 + n_ * 4096, [128, D], F32) for n_ in range(6)]
            NYG = len(YGR)

            def fin(i, th):
                R = RTS[th]
                xr = xres(i)
                for k_ in range(2):
                    n_ = 2 * i + k_
                    yg = YGR[n_ % NYG]
                    idma(lambda e, o=yg.ap, idx=DESTI.ap[:, n_:n_ + 1]:
                         e.indirect_dma_start(out=o, out_offset=None, in_=ys_h[:, :], in_offset=IOA(ap=idx, axis=0),
                                              bounds_check=breg(e, NROWS - 1), oob_is_err=False),
                         reads=[DESTI, Tl(None, ("dram:ys", 0, 64))], writes=[yg])
                    stt(xr, yg, GK[:, n_:n_ + 1], xr, ALU.mult, ALU.add)
                sq_ = YGR[(2 * i) % NYG]
                ot = OUTT[i % 2]
                act(sq_, xr, AF.Square)
                red(R.SS2, sq_, ALU.add)
                act(R.SD2, R.SS2, AF.Sqrt, scale=1.0 / D, bias=EPS_T)
                recip(R.RSTD2, R.SD2)
                stt(ot, xr, R.RSTD2, GFIN, ALU.mult, ALU.mult)
                out_dmas.append(dma(yrow(i), ot))

            for i in range(0, NT, 2):
                P.merge([cap(lambda: fin(i, 0), [0]), cap(lambda: fin(i + 1, 1), [1])])
        sink = P.add("sp", None)
        for i_, o in enumerate(P.ops):
            if o["dma"] and o["fn"] is not None:
                P.ops[sink]["deps"].add(i_)

        n_eng_sems = 4
        from contextlib import ExitStack
        with ExitStack() as es:
            eng_sems = {e: [es.enter_context(nc.semaphore(f"s_{e}{j}")) for j in range(n_eng_sems)]
                        for e in ("pe", "act", "dve", "pool")}
            dma_sems = {"main": [es.enter_context(nc.semaphore(f"s_dma{j}")) for j in range(24)],
                        "cv": [es.enter_context(nc.semaphore(f"s_cv{j}")) for j in range(8)]}
            block = es.enter_context(nc.Block())
            P.emit(nc, block, eng_sems, dma_sems)
    return nc, P


def _consts():
    tri = np.triu(np.ones((128, 128), np.float32))
    pa = np.zeros((4, 128, 128), np.float32)
    pb = np.zeros((4, 128, 128), np.float32)
    pa0 = np.zeros((4, 128, 128), np.float32)
    for g, w in enumerate(POOL_WINDOWS):
        for t in range(128):
            for s in range(max(t - w + 1, -128), t + 1):
                if s >= 0:
                    pa[g, s, t] += 1.0 / w
                else:
                    pb[g, s + 128, t] += 1.0 / w
            pa[g, t, t] -= 1.0
            lo = max(t - w + 1, 0)
            cnt = t + 1 - lo
            for s in range(lo, t + 1):
                pa0[g, s, t] += 1.0 / cnt
            pa0[g, t, t] -= 1.0
    cbf = np.zeros((128, NCBF), np.float32)
    cbf[:, B_ID:B_ID + 128] = np.eye(128, dtype=np.float32)
    cbf[:, B_PA:B_PA + 512] = pa.transpose(1, 0, 2).reshape(128, 512)
    cbf[:, B_PB:B_PB + 512] = pb.transpose(1, 0, 2).reshape(128, 512)
    cbf[:, B_PA0:B_PA0 + 512] = pa0.transpose(1, 0, 2).reshape(128, 512)
    cbf[:, B_TRIS:B_TRIS + 128] = np.triu(np.ones((128, 128), np.float32), 1)
    cbf[:, B_ONESB:B_ONESB + 128] = 1.0
    return tri, cbf.astype(ml_dtypes.bfloat16)


_CACHE = {}


def kernel(x, g_mix, w_in, b_if, conv_q, conv_k, g_head, w_pool, pool_scale, w_br_a, w_br_b,
           w_out, g_ffn, w_rg, b_rg, w_re, b_re, w_e_gate, w_e_up, w_e_down, g_final):
    f = np.float32
    x = np.asarray(x, f)
    tri, cbf = _consts()
    cpk = np.zeros((128, NCPK), f)
    cpk[:, C_GMIX:C_GMIX + 8] = np.asarray(g_mix, f)[0].reshape(8, 128).T
    cpk[:, C_GFFN:C_GFFN + 8] = np.asarray(g_ffn, f)[0].reshape(8, 128).T
    cpk[:, C_PSC:C_PSC + 4] = np.asarray(pool_scale, f)[0].reshape(4, 128).T
    cpk[:, C_GHEAD:C_GHEAD + 4] = np.asarray(g_head, f)[0].reshape(4, 128).T
    cpk[:, C_CQ:C_CQ + 16] = np.asarray(conv_q, f)[0].reshape(4, 4, 128).transpose(2, 1, 0).reshape(128, 16)
    cpk[:, C_CK:C_CK + 16] = np.asarray(conv_k, f)[0].reshape(4, 4, 128).transpose(2, 1, 0).reshape(128, 16)
    cpk[:, C_BIF:C_BIF + 8] = np.asarray(b_if, f)[0][None, :]
    cpk[:, C_BR:C_BR + 4] = np.asarray(b_rg, f)[0][None, :]
    cpk[:, C_BR + 4:C_BR + 36] = np.asarray(b_re, f)[0][None, :]
    cpk[:, C_EPS] = EPS
    cpk[:, C_ONE] = 1.0
    wr = np.concatenate([np.asarray(w_rg, f)[0], np.asarray(w_re, f)[0]], axis=1)
    cpk[:, C_WR:C_WR + 288] = wr.reshape(8, 128, 36).transpose(1, 0, 2).reshape(128, 288)
    cpk[:, C_IDF:C_IDF + 128] = np.eye(128, dtype=f)
    cpk[:, C_TRI:C_TRI + 128] = tri
    cpk[:, C_ONES:C_ONES + 128] = 1.0
    cpk[:, C_GFIN:C_GFIN + 1024] = np.asarray(g_final, f)[None, :]
    cpk[:, C_GFFN2:C_GFFN2 + 8] = np.asarray(g_ffn, f)[0].reshape(128, 8)
    cpk[:, C_PIDX2] = 2.0 * np.arange(128, dtype=f)
    cpk[:, C_THR16:C_THR16 + 16] = 256.0 * np.arange(16, dtype=f)[None, :]
    cpk[:, C_THR48:C_THR48 + 48] = 256.0 * np.arange(48, dtype=f)[None, :]

    key = (STAGE, N_EXPERTS_RUN)
    if key not in _CACHE:
        _CACHE[key] = build_nc()[0]
    nc = _CACHE[key]
    shared = {
        "w_in": np.ascontiguousarray(np.asarray(w_in, f)[0]),
        "w_pool": np.ascontiguousarray(np.asarray(w_pool, f)[0]),
        "w_br_a": np.ascontiguousarray(np.asarray(w_br_a, f)[0]),
        "w_br_b": np.ascontiguousarray(np.asarray(w_br_b, f)[0]),
        "w_out": np.ascontiguousarray(np.asarray(w_out, f)[0]),
        "w_e_gate": np.ascontiguousarray(np.asarray(w_e_gate, f)[0]).reshape(8192, 2048),
        "w_e_up": np.ascontiguousarray(np.asarray(w_e_up, f)[0]).reshape(8192, 2048),
        "w_e_down": np.ascontiguousarray(np.asarray(w_e_down, f)[0]).reshape(8192, 2048),
        "zx": np.zeros((128, 16384), ml_dtypes.bfloat16),
        "cpk": cpk,
        "cbf": cbf,
    }
    in_maps = []
    for c in range(8):
        m = dict(shared)
        m["x"] = np.ascontiguousarray(x[c])
        in_maps.append(m)
    res = run_bass_kernel_spmd(nc, in_maps, core_ids=list(range(8)))
    return np.stack([np.asarray(r["y"], f) for r in res.results], axis=0)
```

```python
import math
import numpy as np
import ml_dtypes
import concourse.bass as bass
import concourse.mybir as mybir
from concourse.bass_utils import run_bass_kernel_spmd

F32 = mybir.dt.float32
BF16 = mybir.dt.bfloat16
I32 = mybir.dt.int32
AF = mybir.ActivationFunctionType
ALU = mybir.AluOpType
AX = mybir.AxisListType

D = 1024
S = 2048
NT = S // 128
NIN = 4616
NE = 32
DH = 128
EPS = 1e-6
POOL_WINDOWS = (2, 4, 8, 16)
STAGE = "full"
N_EXPERTS_RUN = NE

C_GMIX, C_GFFN, C_PSC, C_GHEAD, C_CQ, C_CK, C_BIF, C_BR, C_EPS, C_ONE = 0, 8, 16, 20, 24, 40, 56, 64, 100, 101
C_WR = 102
C_IDF = C_WR + 8 * 36
C_TRI = C_IDF + 128
C_ONES = C_TRI + 128
C_GFIN = C_ONES + 128
C_GFFN2 = C_GFIN + 1024
C_PIDX2 = C_GFFN2 + 8
C_THR16 = C_PIDX2 + 1
C_THR48 = C_THR16 + 16
NCPK = C_THR48 + 48
B_ID, B_PA, B_PB, B_PA0 = 0, 128, 640, 1152
B_TRIS, B_ONESB = 1664, 1792
NCBF = 1920
NBLK = 48
NROWS = NBLK * 256


class Tl:
    __slots__ = ("ap", "reg")

    def __init__(self, ap, reg):
        self.ap = ap
        self.reg = reg

    def __getitem__(self, idx):
        return Tl(self.ap[idx], self.reg)

    def v(self, ap):
        return Tl(ap, self.reg)

    def chunk(self, i, n):
        sp, s0, e0 = self.reg
        w = (e0 - s0) // n
        return Tl(self.ap[:, i], (sp, s0 + i * w, s0 + (i + 1) * w))


class Prog:
    LIMIT = 16000

    def __init__(self):
        self.ops = []
        self.recs = {}
        self._cap = None

    def capture(self, f):
        self._cap = []
        f()
        lst = self._cap
        self._cap = None
        return lst

    def merge(self, threads):
        threads = [t for t in threads if t]
        pos = [0] * len(threads)
        while True:
            best = None
            for k, t in enumerate(threads):
                if pos[k] < len(t):
                    fr = pos[k] / len(t)
                    if best is None or fr < best[0]:
                        best = (fr, k)
            if best is None:
                break
            k = best[1]
            self.add(*threads[k][pos[k]])
            pos[k] += 1

    def add(self, eng, fn, reads=(), writes=(), dma=False, grp="main"):
        if self._cap is not None:
            self._cap.append((eng, fn, tuple(reads), tuple(writes), dma, grp))
            return None
        idx = len(self.ops)
        deps = set()
        for t in reads:
            sp, s, e = t.reg
            for r in self.recs.get(sp, ()):
                if r[3] and r[0] < e and s < r[1]:
                    deps.add(r[2])
        for t in writes:
            sp, s, e = t.reg
            lst = self.recs.get(sp, [])
            keep = []
            for r in lst:
                if r[0] < e and s < r[1]:
                    deps.add(r[2])
                    if s <= r[0] and r[1] <= e:
                        continue
                keep.append(r)
            self.recs[sp] = keep
        tag = eng + ("_dma" if dma else "")
        for t in reads:
            sp, s, e = t.reg
            lst = self.recs.setdefault(sp, [])
            if not dma:
                for r in lst:
                    if (not r[3]) and r[0] == s and r[1] == e and r[4] == tag:
                        r[2] = idx
                        break
                else:
                    lst.append([s, e, idx, False, tag])
            else:
                lst.append([s, e, idx, False, tag])
        for t in writes:
            sp, s, e = t.reg
            self.recs.setdefault(sp, []).append([s, e, idx, True, tag])
        deps.discard(idx)
        self.ops.append(dict(eng=eng, fn=fn, deps=deps, dma=dma, grp=grp))
        return idx

    def emit(self, nc, block, eng_sems, dma_sems):
        ops = self.ops
        n = len(ops)
        dependents = [False] * n
        for i, o in enumerate(ops):
            for d in o["deps"]:
                od = ops[d]
                if od["eng"] == "pe" and o["eng"] == "pe" and not od["dma"] and not o["dma"]:
                    continue
                dependents[d] = True
        sig = [None] * n
        prev_dma = [None] * n
        cnt = {}
        dma_i = {g: 0 for g in dma_sems}
        uses = {g: [0] * len(v) for g, v in dma_sems.items()}
        for i, o in enumerate(ops):
            if o["fn"] is None:
                continue
            if o["dma"]:
                g = o["grp"]
                s = dma_i[g] % len(dma_sems[g])
                dma_i[g] += 1
                uses[g][s] += 1
                sig[i] = (dma_sems[g][s], 16 * uses[g][s])
                if uses[g][s] > 1:
                    prev_dma[i] = (dma_sems[g][s], 16 * (uses[g][s] - 1))
            elif dependents[i]:
                e = o["eng"]
                c = cnt.get(e, 0)
                cnt[e] = c + 1
                sig[i] = (eng_sems[e][c // self.LIMIT], c % self.LIMIT + 1)
        self.counts = cnt

        def make(engname):
            def body(eng):
                waited = {}
                for i, o in enumerate(ops):
                    if o["eng"] != engname:
                        continue
                    waits = []
                    for d in sorted(o["deps"]):
                        if sig[d] is None:
                            continue
                        od = ops[d]
                        if engname == "pe" and od["eng"] == "pe" and not od["dma"] and not o["dma"]:
                            continue
                        waits.append(sig[d])
                    if prev_dma[i] is not None:
                        waits.append(prev_dma[i])
                    for (sm, v) in waits:
                        if waited.get(sm.num, 0) < v:
                            eng.wait_ge(sm, v)
                            waited[sm.num] = v
                    if o["fn"] is not None:
                        ins = o["fn"](eng)
                        if sig[i] is not None:
                            ins.then_inc(sig[i][0], 16 if o["dma"] else 1)
            return body

        block.tensor(make("pe"))
        block.scalar(make("act"))
        block.vector(make("dve"))
        block.gpsimd(make("pool"))
        block.sync(make("sp"))


def build_nc():
    nc = bass.Bass("TRN2", target_bir_lowering=False)
    P = Prog()

    def din(name, shape, dt=F32):
        return nc.dram_tensor(name, list(shape), dt, kind="ExternalInput").ap()

    x_d = din("x", [S, D])
    win_d = din("w_in", [D, NIN])
    wpool_d = din("w_pool", [4, 128, 128])
    wbra_d = din("w_br_a", [512, D])
    wbrb_d = din("w_br_b", [512, D])
    wout_d = din("w_out", [D, D])
    weg_h = nc.dram_tensor("w_e_gate", [8192, 2048], F32, kind="ExternalInput")
    weu_h = nc.dram_tensor("w_e_up", [8192, 2048], F32, kind="ExternalInput")
    wed_h = nc.dram_tensor("w_e_down", [8192, 2048], F32, kind="ExternalInput")
    zx_d = din("zx", [128, 16384], BF16)
    wb_h = [nc.dram_tensor(f"wb{m}", [4096, 4096], BF16) for m in range(3)]
    xs_h = nc.dram_tensor("xs_s", [NROWS, D], BF16)
    ys_h = nc.dram_tensor("ys_s", [NROWS, D], F32)
    cpk_d = din("cpk", [128, NCPK])
    cbf_d = din("cbf", [128, NCBF], BF16)
    y_d = nc.dram_tensor("y", [S, D], F32, kind="ExternalOutput").ap()
    x1_d = nc.dram_tensor("x1s", [S, D], F32).ap()

    def dreg(name, ap, lo, hi):
        return Tl(ap, ("dram:" + name, lo, hi))

    ARENA_BYTES = 212480
    with (
        nc.sbuf_tensor("arena", [128, ARENA_BYTES // 4], F32) as arena,
        nc.psum_tensor("ps", [128, 8, 512], F32) as ps,
    ):
        def sview(off, shape, dt):
            esz = 4 if dt in (F32, I32) else 2
            nel = 1
            for s_ in shape[1:]:
                nel *= s_
            nb = nel * esz
            assert off % 4 == 0 and nb % 4 == 0, (off, shape)
            assert off + nb <= ARENA_BYTES, (off, nb)
            ap = arena[:, off // 4:(off + nb) // 4]
            if dt != F32:
                ap = ap.bitcast(dt)
            if len(shape) == 3:
                ap = ap.rearrange("p (a b) -> p a b", a=shape[1])
            return Tl(ap, ("sb", off, off + nb))

        class Bump:
            def __init__(self, lo, hi):
                self.o = lo
                self.hi = hi

            def __call__(self, shape, dt):
                esz = 4 if dt in (F32, I32) else 2
                nel = 1
                for s_ in shape[1:]:
                    nel *= s_
                nb = (nel * esz + 3) // 4 * 4
                t = sview(self.o, shape, dt)
                self.o += nb
                assert self.o <= self.hi, (self.o, self.hi)
                return t

        psi = [0]
        bpool = [list(range(8))]

        def bank():
            pool = bpool[0]
            b = pool[psi[0] % len(pool)]
            psi[0] += 1
            return Tl(ps[:, b, :], ("ps", b * 2048, (b + 1) * 2048))

        def cap(f, banks):
            old = bpool[0]
            bpool[0] = banks
            lst = P.capture(f)
            bpool[0] = old
            return lst

        def bf_view(bk, a, b):
            return bk.v(bk.ap.bitcast(BF16)[:, 0:a * b].rearrange("p (a b) -> p a b", a=a))

        def f_view(bk, a, b):
            return bk.v(bk.ap[:, 0:a * b].rearrange("p (a b) -> p a b", a=a))

        def mm(out, lhsT, rhs, start, stop):
            P.add("pe", lambda e, o=out.ap, l=lhsT.ap, r=rhs.ap, st=start, sp=stop:
                  e.matmul(o, l, r, start=st, stop=sp), reads=[lhsT, rhs], writes=[out])

        def tr(out, in_, ident):
            P.add("pe", lambda e, o=out.ap, i=in_.ap, d=ident.ap: e.transpose(o, i, d),
                  reads=[in_, ident], writes=[out])

        def act(out, in_, func, scale=1.0, bias=None):
            rd = [in_] + ([bias] if bias is not None else [])
            if bias is None:
                P.add("act", lambda e, o=out.ap, i=in_.ap, f=func, s=scale:
                      e.activation(o, i, f, scale=s), reads=rd, writes=[out])
            else:
                P.add("act", lambda e, o=out.ap, i=in_.ap, f=func, s=scale, b=bias.ap:
                      e.activation(o, i, f, bias=b, scale=s), reads=rd, writes=[out])

        def tt(out, a, b, op, eng="dve"):
            P.add(eng, lambda e, o=out.ap, x=a.ap, y=b.ap, p=op: e.tensor_tensor(o, x, y, p),
                  reads=[a, b], writes=[out])

        def ts(out, a, s1, s2, op0, op1=None, eng="dve"):
            rd = [a] + [s for s in (s1, s2) if isinstance(s, Tl)]
            v1 = s1.ap if isinstance(s1, Tl) else s1
            v2 = s2.ap if isinstance(s2, Tl) else s2
            if op1 is None:
                P.add(eng, lambda e, o=out.ap, x=a.ap, p=op0: e.tensor_scalar(o, x, v1, None, p),
                      reads=rd, writes=[out])
            else:
                P.add(eng, lambda e, o=out.ap, x=a.ap, p=op0, q=op1: e.tensor_scalar(o, x, v1, v2, p, q),
                      reads=rd, writes=[out])

        def stt(out, a, sc, b, op0, op1):
            rd = [a, b] + ([sc] if isinstance(sc, Tl) else [])
            sv = sc.ap if isinstance(sc, Tl) else sc
            P.add("dve", lambda e, o=out.ap, x=a.ap, y=b.ap, p=op0, q=op1:
                  e.scalar_tensor_tensor(o, x, sv, y, p, q), reads=rd, writes=[out])

        def red(out, in_, op):
            P.add("dve", lambda e, o=out.ap, i=in_.ap, p=op: e.tensor_reduce(o, i, AX.X, p),
                  reads=[in_], writes=[out])

        def recip(out, in_):
            P.add("dve", lambda e, o=out.ap, i=in_.ap: e.reciprocal(o, i), reads=[in_], writes=[out])

        def cp(out, in_, eng="dve"):
            if eng == "act":
                P.add("act", lambda e, o=out.ap, i=in_.ap: e.copy(o, i), reads=[in_], writes=[out])
            else:
                P.add(eng, lambda e, o=out.ap, i=in_.ap: e.tensor_copy(o, i), reads=[in_], writes=[out])

        def memset(out, val, eng="pool"):
            P.add(eng, lambda e, o=out.ap, v=val: e.memset(o, v), writes=[out])

        def dma(out, in_):
            return P.add("sp", lambda e, o=out.ap, i=in_.ap: e.dma_start(out=o, in_=i),
                         reads=[in_], writes=[out], dma=True)

        def idma(fn, reads, writes, grp="sw"):
            return P.add("pool", fn, reads=reads, writes=writes, dma=True, grp=grp)

        def bc(t, shape, axis):
            return t.v(t.ap.unsqueeze(axis).to_broadcast(shape))

        CB = Bump(0, 11328)
        CPK = CB([128, NCPK], F32)
        CBF = CB([128, NCBF], BF16)
        X0 = 11328
        W0 = X0 + 65536
        S0 = W0 + 107648
        assert S0 == 184512

        def cslice(c0, n):
            return CPK[:, c0:c0 + n]

        GMIX = cslice(C_GMIX, 8)
        GFFN = cslice(C_GFFN, 8)
        PSC = cslice(C_PSC, 4)
        GHEAD = cslice(C_GHEAD, 4)
        CQ = CPK.v(CPK.ap[:, C_CQ:C_CQ + 16].rearrange("p (h j) -> p h j", h=4))
        CK = CPK.v(CPK.ap[:, C_CK:C_CK + 16].rearrange("p (h j) -> p h j", h=4))
        BIF = cslice(C_BIF, 8)
        BR = cslice(C_BR, 36)
        EPS_T = cslice(C_EPS, 1)
        ONE_T = cslice(C_ONE, 1)
        WR = CPK.v(CPK.ap[:, C_WR:C_WR + 288].rearrange("p (k n) -> p k n", k=8))
        IDF = cslice(C_IDF, 128)
        TRIF = cslice(C_TRI, 128)
        ONESF = cslice(C_ONES, 128)
        GFIN = cslice(C_GFIN, 1024)
        IDB = CBF[:, B_ID:B_ID + 128]
        PA = CBF.v(CBF.ap[:, B_PA:B_PA + 512].rearrange("p (g t) -> p g t", g=4))
        PB = CBF.v(CBF.ap[:, B_PB:B_PB + 512].rearrange("p (g t) -> p g t", g=4))
        PA0 = CBF.v(CBF.ap[:, B_PA0:B_PA0 + 512].rearrange("p (g t) -> p g t", g=4))
        TRIS = CBF[:, B_TRIS:B_TRIS + 128]
        ONESB = CBF[:, B_ONESB:B_ONESB + 128]
        GFFN2 = cslice(C_GFFN2, 8)
        PIDX2 = cslice(C_PIDX2, 1)
        THR16 = cslice(C_THR16, 16)
        THR48 = cslice(C_THR48, 48)

        dma(CPK, dreg("cpk", cpk_d, 0, 1))
        dma(CBF, dreg("cbf", cbf_d, 0, 1))

        WB = Bump(W0, S0)
        WIN = WB([128, 8, NIN], BF16)
        WPOOL = WB([128, 4, 128], BF16)
        WBRA = WB([128, 4, D], BF16)
        WBRB = WB([128, 4, D], BF16)
        WOUT = WB([128, 8, D], BF16)

        XB_ = Bump(X0, X0 + 65536)
        SBX = Bump(S0, ARENA_BYTES)
        XT = [XB_([128, D], F32) for _ in range(2)] + [SBX([128, D], F32), SBX([128, D], F32)]
        XS = XB_([128, D], BF16)
        HT_ = XB_([128, 8, 128], BF16)
        U = [XB_([128, 512], BF16) for _ in range(2)]
        VAUG = [XB_([128, 4, 130], BF16), SBX([128, 4, 130], BF16)]
        QC = XB_([128, 4, 132], F32)
        KC = XB_([128, 4, 132], F32)
        QACC = XB_([128, 4, 128], F32)
        KACC = XB_([128, 4, 128], F32)
        QT = [XB_([128, 4, 128], BF16), SBX([128, 4, 128], BF16)]
        KT = [XB_([128, 4, 128], BF16), SBX([128, 4, 128], BF16)]
        KP = [XB_([128, 4, 128], BF16), SBX([128, 4, 128], BF16)]
        SDT = XB_([128, 4, 128], BF16)
        C32 = XB_([128, 4, 129], F32)
        CBFS = XB_([128, 4, 130], BF16)
        SIGO = [XB_([128, 512], F32), SBX([128, 512], F32)]
        SGA = XB_([128, D], BF16)
        SGB = [XB_([128, D], BF16), SBX([128, D], BF16)]
        DT_ = XB_([128, 4, 128], BF16)
        YPT = XB_([128, 4, 128], BF16)
        HN = XB_([128, 4, 128], F32)
        HNO = XB_([128, 512], BF16)
        HMT = XB_([128, 4, 128], BF16)
        SMS = [XB_([128, 128], F32), SBX([128, 128], F32)]
        M1 = [XB_([128, D], F32), SBX([128, D], F32)]
        X1T = [XB_([128, D], F32), SBX([128, D], F32)]
        MRG = XB_([128, D], BF16)
        MT_ = XB_([128, 8, 128], BF16)
        TMPQK = sview(QACC.reg[1], [128, D], F32)
        assert KACC.reg[1] == QACC.reg[2]
        STG_N = 8
        STG = [sview(X0 + 65536 - (j + 1) * 4616, [128, 1154], F32) for j in range(STG_N)]

        class SmallSet:
            pass

        SMV = []
        for SM in SMS:
            smi = [0]

            def sm(n, SM=SM, smi=smi):
                t = SM[:, smi[0]:smi[0] + n]
                smi[0] += n
                assert smi[0] <= 128
                return t

            v_ = SmallSet()
            v_.SS, v_.SD_, v_.RSTD = sm(1), sm(1), sm(1)
            v_.GIF, v_.E1, v_.L1, v_.TMP4, v_.RR, v_.EB, v_.EBL = sm(8), sm(4), sm(4), sm(4), sm(4), sm(4), sm(4)
            (v_.NQV, v_.DEN, v_.RDEN, v_.FAC, v_.F2, v_.VH, v_.SDH, v_.RSH, v_.SC) = (sm(4) for _ in range(9))
            v_.ST6 = SM.v(sm(24).ap.rearrange("p (h s) -> p h s", h=4))
            v_.MV = SM.v(sm(8).ap.rearrange("p (h s) -> p h s", h=4))
            SMV.append(v_)

        cast_engs = ["act", "dve"]
        pcs = [0]

        def load_cast(dst, src_ap, src_reg, ncols):
            j = pcs[0]
            pcs[0] += 1
            st = STG[j % STG_N]
            stv = st[:, 0:ncols]
            dma(stv, dreg(*src_reg).v(src_ap))
            cp(dst, stv, eng=cast_engs[j % 2])

        for k in range(8):
            for c in range(4):
                load_cast(WIN[:, k, c * 1154:(c + 1) * 1154],
                          win_d[k * 128:(k + 1) * 128, c * 1154:(c + 1) * 1154],
                          ("w_in", win_d, 0, 1), 1154)
        st = STG[pcs[0] % STG_N]
        pcs[0] += 1
        stv = st.v(st.ap[:, 0:512].rearrange("p (g d) -> p g d", g=4))
        dma(stv, dreg("w_pool", wpool_d.rearrange("g c d -> c g d"), 0, 1))
        cp(WPOOL, stv, eng="dve")
        for g in range(4):
            load_cast(WBRA[:, g, :], wbra_d[g * 128:(g + 1) * 128, :], ("w_br_a", wbra_d, 0, 1), 1024)
        for g in range(4):
            load_cast(WBRB[:, g, :], wbrb_d[g * 128:(g + 1) * 128, :], ("w_br_b", wbrb_d, 0, 1), 1024)
        for k in range(8):
            load_cast(WOUT[:, k, :], wout_d[k * 128:(k + 1) * 128, :], ("w_out", wout_d, 0, 1), 1024)

        xs_flat = xs_h.ap().rearrange("(a b) f -> a (b f)", b=16)

        def zero_fill(c):
            dma(Tl(xs_flat[c * 128:(c + 1) * 128, :], ("dram:xs", 0, 64)), dreg("zx", zx_d, 0, 1))
        for b_ in range(2):
            memset(VAUG[b_], 1.0)
        memset(QC, 0.0)
        memset(KC, 0.0)
        memset(C32, 0.0)
        memset(CBFS, 0.0)

        LNS = math.log(DH ** -0.5)

        def xrow(i):
            return dreg("x", x_d[i * 128:(i + 1) * 128, :], i, i + 1)

        def x1row(i):
            return dreg("x1s", x1_d[i * 128:(i + 1) * 128, :], i, i + 1)

        def yrow(i):
            return dreg("y", y_d[i * 128:(i + 1) * 128, :], i, i + 1)

        def proj(col0, ncols):
            pb = bank()
            o = pb[:, 0:ncols]
            for k in range(8):
                mm(o, HT_[:, k, :], WIN[:, k, col0:col0 + ncols], k == 0, k == 7)
            return o

        def A1(i):
            b = i % 2
            xt = XT[i % 4]
            V = SMV[b]
            act(TMPQK, xt, AF.Square)
            red(V.SS, TMPQK, ALU.add)
            act(V.SD_, V.SS, AF.Sqrt, scale=1.0 / D, bias=EPS_T)
            recip(V.RSTD, V.SD_)
            ts(XS, xt, V.RSTD, None, ALU.mult)
            pt = bank()
            ptb = bf_view(pt, 8, 128)
            for k in range(8):
                tr(ptb[:, k, :], XS[:, k * 128:(k + 1) * 128], IDB)
            tt(HT_, ptb, bc(GMIX, [128, 8, 128], 2), ALU.mult)
            pu = proj(0, 512)
            cp(U[b], pu, eng="act")
            pv = proj(1536, 512)
            cp(VAUG[b][:, :, 0:128], pv.v(pv.ap.rearrange("p (h d) -> p h d", h=4)), eng="dve")
            po = proj(2048, 512)
            act(SIGO[b], po, AF.Sigmoid)
            pif = proj(2560, 8)
            tt(V.GIF, pif, BIF, ALU.add)
            act(V.E1, V.GIF[:, 4:8], AF.Exp, scale=-1.0)
            act(V.L1, V.E1, AF.Ln, bias=ONE_T)
            pgt = bank()
            mm(pgt[:, 0:4], TRIF, V.L1, True, True)
            mm(pgt[:, 4:8], ONESF, V.L1, True, True)
            stt(V.TMP4, V.GIF[:, 0:4], LNS, pgt[:, 0:4], ALU.add, ALU.add)
            act(V.RR, V.TMP4, AF.Exp)
            act(V.EB, pgt[:, 0:4], AF.Exp, scale=-1.0)
            act(V.EBL, pgt[:, 4:8], AF.Exp, scale=-1.0)

        def A2(i):
            b = i % 2
            ucur, uprev = U[b], U[(i + 1) % 2]
            for hf in range(2):
                pg_ = proj(2568 + hf * 512, 512)
                act(SGA[:, hf * 512:(hf + 1) * 512], pg_, AF.Sigmoid)
            for hf in range(2):
                pg_ = proj(3592 + hf * 512, 512)
                act(SGB[b][:, hf * 512:(hf + 1) * 512], pg_, AF.Sigmoid)
            pd = bank()
            pdv = f_view(pd, 4, 128)
            for g in range(4):
                mm(pdv[:, g, :], ucur[:, g * 128:(g + 1) * 128], (PA0 if i == 0 else PA)[:, g, :], True, i == 0)
                if i > 0:
                    mm(pdv[:, g, :], uprev[:, g * 128:(g + 1) * 128], PB[:, g, :], False, True)
            cp(DT_, pdv, eng="act")
            py = bank()
            pyv = f_view(py, 4, 128)
            for g in range(4):
                mm(pyv[:, g, :], WPOOL[:, g, :], DT_[:, g, :], True, True)
            tt(YPT, pyv, bc(PSC, [128, 4, 128], 2), ALU.mult)
            pa = [bank() for _ in range(2)]
            for hf in range(2):
                for g in range(4):
                    mm(pa[hf], YPT[:, g, :], WBRA[:, g, hf * 512:(hf + 1) * 512], g == 0, g == 3)
            for hf in range(2):
                sl = slice(hf * 512, (hf + 1) * 512)
                tt(M1[b][:, sl], pa[hf], SGA[:, sl], ALU.mult)

        def A3(i):
            b = i % 2
            V = SMV[b]
            for (col0, buf) in ((512, QC), (1024, KC)):
                pq = bank()
                pqv = f_view(pq, 4, 128)
                for h in range(4):
                    for k in range(8):
                        mm(pqv[:, h, :], WIN[:, k, col0 + h * 128:col0 + (h + 1) * 128], HT_[:, k, :],
                           k == 0, k == 7)
                cp(buf[:, :, 3:131], pqv, eng="act")
            TMPQ = TMPQK.v(TMPQK.ap[:, 0:512].rearrange("p (h t) -> p h t", h=4))
            TMPK = TMPQK.v(TMPQK.ap[:, 512:1024].rearrange("p (h t) -> p h t", h=4))
            for (buf, acc, cw, tmp, outT) in ((QC, QACC, CQ, TMPQ, QT[b]), (KC, KACC, CK, TMPK, KT[b])):
                for j in range(4):
                    for h in range(4):
                        ah = acc.chunk(h, 4)
                        if j == 0:
                            ts(ah, buf[:, h, 0:128], cw[:, h, 0:1], None, ALU.mult)
                        else:
                            stt(ah, buf[:, h, j:j + 128], cw[:, h, j:j + 1], ah, ALU.mult, ALU.add)
                act(outT, acc, AF.Silu)
                cp(buf[:, :, 0:3], buf[:, :, 128:131], eng="dve")
            pk = bank()
            pkb = bf_view(pk, 4, 128)
            for h in range(4):
                tr(pkb[:, h, :], KT[b][:, h, :], IDB)
            tt(KP[b], pkb, bc(V.RR, [128, 4, 128], 2), ALU.mult)

        def B1(i):
            b = i % 2
            V = SMV[b]
            psb = bank()
            psv = f_view(psb, 4, 128)
            for h in range(4):
                mm(psv[:, h, :], KT[b][:, h, :], QT[b][:, h, :], True, True)
            for h in range(4):
                stt(SDT[:, h, :], psv[:, h, :], V.RR[:, h:h + 1], TRIF, ALU.mult, ALU.mult)
            pn = [f_view(bank(), 2, 129) for _ in range(2)]
            for h in range(4):
                o = pn[h // 2][:, h % 2, :]
                mm(o, SDT[:, h, :], VAUG[b][:, h, 0:129], True, i == 0)
                if i > 0:
                    mm(o, QT[b][:, h, :], CBFS[:, h, 0:129], False, True)
            if i + 1 < NT:
                pc = [f_view(bank(), 2, 129) for _ in range(2)]
                for h in range(4):
                    mm(pc[h // 2][:, h % 2, :], KP[b][:, h, :], VAUG[b][:, h, 0:129], True, True)
            for p_ in range(2):
                tt(V.NQV[:, 2 * p_:2 * p_ + 2], pn[p_][:, :, 128], V.EB[:, 2 * p_:2 * p_ + 2], ALU.mult)
            ts(V.RDEN, V.NQV, -1.0, None, ALU.mult)
            tt(V.DEN, V.NQV, V.RDEN, ALU.max)
            ts(V.DEN, V.DEN, 1.0, None, ALU.max)
            recip(V.RDEN, V.DEN)
            tt(V.FAC, V.EB, V.RDEN, ALU.mult)
            for h in range(4):
                P.add("dve", lambda e, o=V.ST6.ap[:, h, :], i_=pn[h // 2].ap[:, h % 2, 0:128]: e.bn_stats(o, i_),
                      reads=[pn[h // 2]], writes=[V.ST6])
                P.add("dve", lambda e, o=V.MV.ap[:, h, :], i_=V.ST6.ap[:, h, :]: e.bn_aggr(o, i_),
                      reads=[V.ST6], writes=[V.MV])
            tt(V.F2, V.FAC, V.FAC, ALU.mult)
            tt(V.VH, V.F2, V.MV[:, :, 1], ALU.mult)
            act(V.SDH, V.VH, AF.Sqrt, bias=EPS_T)
            recip(V.RSH, V.SDH)
            tt(V.SC, V.FAC, V.RSH, ALU.mult)
            for h in range(4):
                ts(HN[:, h, :], pn[h // 2][:, h % 2, 0:128], V.MV[:, h, 0:1], V.SC[:, h:h + 1],
                   ALU.subtract, ALU.mult)
            tt(HNO, HN.v(HN.ap.rearrange("p h d -> p (h d)")), SIGO[b], ALU.mult)
            if i + 1 < NT:
                for p_ in range(2):
                    tt(C32[:, 2 * p_:2 * p_ + 2, :], C32[:, 2 * p_:2 * p_ + 2, :], pc[p_], ALU.add)
                    tt(C32[:, 2 * p_:2 * p_ + 2, :], C32[:, 2 * p_:2 * p_ + 2, :],
                       V.EBL.v(V.EBL.ap[:, 2 * p_:2 * p_ + 2].unsqueeze(2).to_broadcast([128, 2, 129])), ALU.mult)
                cp(CBFS[:, :, 0:129], C32, eng="act")

        def B2a(i):
            b = i % 2
            ph = bank()
            phb = bf_view(ph, 4, 128)
            for h in range(4):
                tr(phb[:, h, :], HNO[:, h * 128:(h + 1) * 128], IDB)
            tt(HMT, phb, bc(GHEAD, [128, 4, 128], 2), ALU.mult)
            pbb = [bank() for _ in range(2)]
            for hf in range(2):
                for h in range(4):
                    mm(pbb[hf], HMT[:, h, :], WBRB[:, h, hf * 512:(hf + 1) * 512], h == 0, h == 3)
            for hf in range(2):
                sl = slice(hf * 512, (hf + 1) * 512)
                tt(X1T[b][:, sl], pbb[hf], SGB[b][:, sl], ALU.mult)

        def B2b(i):
            b = i % 2
            xt = XT[i % 4]
            tt(MRG, M1[b], X1T[b], ALU.add)
            pm = bank()
            pmb = bf_view(pm, 8, 128)
            for k in range(8):
                tr(pmb[:, k, :], MRG[:, k * 128:(k + 1) * 128], IDB)
            cp(MT_, pmb, eng="act")
            pox = [bank() for _ in range(2)]
            for hf in range(2):
                for k in range(8):
                    mm(pox[hf], MT_[:, k, :], WOUT[:, k, hf * 512:(hf + 1) * 512], k == 0, k == 7)
            for hf in range(2):
                sl = slice(hf * 512, (hf + 1) * 512)
                tt(X1T[b][:, sl], pox[hf], xt[:, sl], ALU.add)
            dma(yrow(i) if STAGE == "mixer" else x1row(i), X1T[b])
            if i + 4 < NT:
                dma(XT[i % 4], xrow(i + 4))
            if STAGE != "mixer" and i % 2 == 1 and i // 2 < NROWS // 16 // 128:
                zero_fill(i // 2)

        dma(XT[0], xrow(0))
        dma(XT[1], xrow(1))
        dma(XT[2], xrow(2))
        dma(XT[3], xrow(3))
        WSRC = (weg_h, weu_h, wed_h)
        NCV = 32

        def conv_w(n_, gate=None):
            m_, c = divmod(n_, NCV)
            if m_ < 2:
                r0, r1 = c * 8192 // NCV, (c + 1) * 8192 // NCV
                dst_ap = wb_h[m_].ap().rearrange("r (h m) -> (r h) m", h=2)[r0:r1, :]
                src_ap = WSRC[m_][r0:r1, :]
            else:
                assert NCV == 32
                src_ap = WSRC[2].ap().rearrange("r (h m) -> (r h) m", h=2)[c * 512:(c + 1) * 512, :].rearrange(
                    "(k p) n -> p k n", p=128)
                dst_ap = wb_h[2].ap().rearrange("(e p) (k n) -> e p k n", p=128, k=4)[c]
            rd = [Tl(None, ("fake:cv", n_ - 4, n_ - 3))] if n_ >= 4 else []
            if gate is not None:
                rd.append(gate)
            idma(lambda e, o=dst_ap, src=src_ap: e.dma_start(out=o, in_=src),
                 reads=rd, writes=[Tl(None, ("dram:wb%d" % m_, c, c + 1)), Tl(None, ("fake:cv", n_, n_ + 1))],
                 grp="cv")
        A1(0)
        if STAGE != "mixer":
            for n_ in range(3 * NCV):
                conv_w(n_, gate=(XS if n_ == 0 else None))
        P.merge([cap(lambda: A3(0), [0, 1, 2, 3]), cap(lambda: A2(0), [4, 5, 6, 7])])
        for i in range(NT):
            nx = i + 1 < NT
            th = [cap(lambda: B1(i), [0, 1, 2, 3, 4])]
            if nx:
                th.append(cap(lambda: A1(i + 1), [5, 6, 7]))
            P.merge(th)
            th = [cap(lambda: (B2a(i), B2b(i)), [0, 1, 2])]
            if nx:
                th.append(cap(lambda: A3(i + 1), [3, 4]))
                th.append(cap(lambda: A2(i + 1), [5, 6, 7]))
            P.merge(th)

        out_dmas = []
        if STAGE != "mixer":
            def xres(i, hf=None):
                if hf is None:
                    return sview(X0 + i * 4096, [128, D], F32)
                return sview(X0 + i * 4096 + hf * 2048, [128, 512], F32)

            MB = Bump(W0, S0)
            XSB = MB([128, NT, D], BF16)
            WG = [MB([128, 8, 512], BF16) for _ in range(2)]
            WU = [MB([128, 8, 512], BF16) for _ in range(2)]
            WD = [MB([128, 4, D], BF16) for _ in range(2)]
            ESTG = [MB([128, 2048], F32) for _ in range(3)]

            def sv(off, shape, dt):
                return sview(S0 + off, shape, dt)

            DESTI = sv(0, [128, 32], I32)
            GK = sv(128, [128, 32], F32)
            IDXI = sv(256, [128, NBLK], I32)
            E0 = ESTG[0].reg[1]
            RSA = [sv(640, [128, 160], F32), sv(1280, [128, 160], F32),
                   sview(E0 + 16384, [128, 160], F32), sview(E0 + 17024, [128, 160], F32)]
            RS = sv(1920, [128, 416], F32)
            OHK = sv(3584, [128, NT * 2 * 32], F32)
            OHSB = sv(7680, [128, NT, 32], BF16)
            RANK = sv(8704, [128, NT, 32], F32)
            XS32 = [sv(10752, [128, D], F32), sv(18944, [128, D], F32),
                    sview(E0, [128, D], F32), sview(E0 + 8192, [128, D], F32)]
            HF32 = [sv(14848, [128, 8, 128], F32), sv(23040, [128, 8, 128], F32),
                    sview(E0 + 4096, [128, 8, 128], F32), sview(E0 + 12288, [128, 8, 128], F32)]
            TMPD = sv(10752, [128, NT * 2 * 32], F32)
            TMPB = sv(14848, [128, NBLK * 32], F32)
            NB16 = sv(20992, [128, 32 * 16], F32)
            XB = sv(3584, [128, 2, D], BF16)
            XTB = sv(7680, [128, 8, 256], BF16)
            HTB = [sv(11776, [128, 4, 256], BF16), sv(13824, [128, 4, 256], BF16)]
            SGb = [sv(15872, [128, 256], F32), sv(16896, [128, 256], F32)]
            YB = sv(17920, [128, 2, D], F32)
            YG = [sv(3584 + n_ * 4096, [128, D], F32) for n_ in range(3)]
            OUTT = [sv(15872, [128, D], F32), sv(19968, [128, D], F32)]
            assert S0 + 27136 <= ARENA_BYTES
            rsi = [0]

            def rs(n):
                t = RS[:, rsi[0]:rsi[0] + n]
                rsi[0] += n
                assert rsi[0] <= 416
                return t

            class RT:
                pass

            RTS = []
            for RSx in RSA:
                c_ = [0]

                def ra(n, RSx=RSx, c_=c_):
                    t = RSx[:, c_[0]:c_[0] + n]
                    c_[0] += n
                    assert c_[0] <= 160
                    return t

                r_ = RT()
                r_.SS2, r_.SD2, r_.RSTD2 = ra(1), ra(1), ra(1)
                r_.LG = ra(36)
                r_.GMAX, r_.OHG, r_.GSH, r_.GE, r_.GSUM, r_.PGV = ra(1), ra(4), ra(4), ra(4), ra(1), ra(1)
                r_.T32 = RSx.v(ra(32).ap.rearrange("p (g j) -> p g j", g=4))
                r_.ESEL, r_.M1_, r_.OH1, r_.MSK, r_.M2_, r_.OH2 = ra(8), ra(1), ra(8), ra(8), ra(1), ra(8)
                r_.DD, r_.E2, r_.DEN2, r_.W1, r_.W2 = (ra(1) for _ in range(5))
                RTS.append(r_)
            SS2, SD2, RSTD2 = RTS[0].SS2, RTS[0].SD2, RTS[0].RSTD2
            CUM, NBLKT, PADA, CSA, CSB_, POFF = (rs(32) for _ in range(6))
            BEF, EMP, IDXF, IDXF1 = (rs(NBLK) for _ in range(4))
            DESTF = rs(32)
            ohk4 = OHK.v(OHK.ap.rearrange("p (i k e) -> p i k e", i=NT, k=2))

            def sub(t, ap, off, nb):
                sp_, s0_, _ = t.reg
                return Tl(ap, (sp_, s0_ + off, s0_ + off + nb))

            for i in range(NT):
                dma(xres(i), x1row(i))

            def route(i, th):
                R = RTS[th]
                xs32, hf32 = XS32[th], HF32[th]
                sqs = hf32.v(hf32.ap.rearrange("p k t -> p (k t)"))
                xi = xres(i)
                act(sqs, xi, AF.Square)
                red(R.SS2, sqs, ALU.add)
                act(R.SD2, R.SS2, AF.Sqrt, scale=1.0 / D, bias=EPS_T)
                recip(R.RSTD2, R.SD2)
                ts(xs32, xi, R.RSTD2, None, ALU.mult)
                cp(sub(XSB, XSB.ap[:, i, :], i * 2048, 2048), xs32, eng="act")
                pp = [bank() for _ in range(2)]
                for k in range(8):
                    tr(pp[k // 4][:, (k % 4) * 128:(k % 4 + 1) * 128], xs32[:, k * 128:(k + 1) * 128], IDF)
                for hf in range(2):
                    tt(hf32[:, 4 * hf:4 * hf + 4, :], f_view(pp[hf], 4, 128),
                       GFFN.v(GFFN.ap[:, 4 * hf:4 * hf + 4].unsqueeze(2).to_broadcast([128, 4, 128])), ALU.mult)
                pr = bank()
                for k in range(8):
                    mm(pr[:, 0:36], hf32[:, k, :], WR[:, k, :], k == 0, k == 7)
                lg4 = LG4B[(i // 4) % 2]
                tt(sub(lg4, lg4.ap[:, th, :], th * 144, 144), pr[:, 0:36], BR, ALU.add)

            BT = sview(E0 + 17664, [128, 768], F32)
            bti = [0]

            def bt(n, shape=None):
                t = BT[:, bti[0]:bti[0] + n]
                bti[0] += n
                assert bti[0] <= 768
                if shape is not None:
                    t = t.v(t.ap.rearrange("p (a b) -> p a b", a=shape[0]))
                return t

            LG4B = [bt(144, (4, 36)), bt(144, (4, 36))]
            GMAX4, GSUM4, PGV4, M14, M24, DD4, E24, DEN4, W14, W24 = (bt(4) for _ in range(10))
            OHG4, GSH4, GE4 = bt(16, (4, 4)), bt(16, (4, 4)), bt(16, (4, 4))
            T324 = bt(128)
            ESEL4, OH14, MSK4, OH24 = (bt(32, (4, 8)) for _ in range(4))

            def b3(t, shape):
                return t.v(t.ap.unsqueeze(2).to_broadcast(shape))

            def route_small(g0):
                LG4 = LG4B[(g0 // 4) % 2]
                gl = LG4[:, :, 0:4]
                el = LG4.v(LG4.ap[:, :, 4:36].rearrange("p t (g j) -> p t g j", g=4))
                red(GMAX4, gl, ALU.max)
                tt(OHG4, gl, b3(GMAX4, [128, 4, 4]), ALU.is_equal)
                tt(GSH4, gl, b3(GMAX4, [128, 4, 4]), ALU.subtract)
                act(GE4, GSH4, AF.Exp)
                red(GSUM4, GE4, ALU.add)
                recip(PGV4, GSUM4)
                t4 = T324.v(T324.ap.rearrange("p (t g j) -> p t g j", t=4, g=4))
                tt(t4, el, OHG4.v(OHG4.ap.unsqueeze(3).to_broadcast([128, 4, 4, 8])), ALU.mult)
                red(ESEL4, T324.v(T324.ap.rearrange("p (t g j) -> p t j g", t=4, g=4)), ALU.add)
                red(M14, ESEL4, ALU.max)
                tt(OH14, ESEL4, b3(M14, [128, 4, 8]), ALU.is_equal)
                stt(MSK4, OH14, -1e30, ESEL4, ALU.mult, ALU.add)
                red(M24, MSK4, ALU.max)
                tt(OH24, MSK4, b3(M24, [128, 4, 8]), ALU.is_equal)
                tt(DD4, M24, M14, ALU.subtract)
                act(E24, DD4, AF.Exp)
                ts(DEN4, E24, 1.0, None, ALU.add)
                recip(W14, DEN4)
                tt(W24, E24, W14, ALU.mult)
                gk4 = GK.v(GK.ap[:, 2 * g0:2 * g0 + 8].rearrange("p (t k) -> p t k", k=2))
                tt(gk4[:, :, 0], PGV4, W14, ALU.mult)
                tt(gk4[:, :, 1], PGV4, W24, ALU.mult)
                ohg_b = OHG4.v(OHG4.ap.unsqueeze(3).to_broadcast([128, 4, 4, 8]))
                for k_, oh in ((0, OH14), (1, OH24)):
                    dst = OHK.v(ohk4.ap[:, g0:g0 + 4, k_, :].rearrange("p t (g j) -> p t g j", g=4))
                    tt(dst, ohg_b, oh.v(oh.ap.unsqueeze(2).to_broadcast([128, 4, 4, 8])), ALU.mult)
                tt(OHSB[:, g0:g0 + 4, :], ohk4[:, g0:g0 + 4, 0, :], ohk4[:, g0:g0 + 4, 1, :], ALU.add)

            for i in range(0, NT, 4):
                th = [cap(lambda: route(i + t_, t_), [2 * t_, 2 * t_ + 1]) for t_ in range(4)]
                if i >= 4:
                    th.append(cap(lambda: route_small(i - 4), [0]))
                P.merge(th)
            route_small(NT - 4)

            prk = bank()
            pct = bank()
            for i in range(NT):
                o = prk[:, i * 32:(i + 1) * 32]
                mm(o, TRIS, OHSB[:, i, :], True, i == 0)
                for i2 in range(i):
                    mm(o, ONESB, OHSB[:, i2, :], False, i2 == i - 1)
            for i in range(NT):
                mm(pct[:, 0:32], ONESB, OHSB[:, i, :], i == 0, i == NT - 1)
            cp(CUM, pct[:, 0:32])
            nb16 = NB16.v(NB16.ap.rearrange("p (e m) -> p e m", m=16))
            tt(nb16, CUM.v(CUM.ap.unsqueeze(2).to_broadcast([128, 32, 16])),
               THR16.v(THR16.ap.unsqueeze(1).to_broadcast([128, 32, 16])), ALU.is_gt)
            red(NBLKT, nb16, ALU.add)
            ts(PADA, NBLKT, 256.0, None, ALU.mult)
            cp(CSA, PADA)
            cur, nxt = CSA, CSB_
            for sh in (1, 2, 4, 8, 16):
                cp(nxt[:, 0:sh], cur[:, 0:sh])
                tt(nxt[:, sh:32], cur[:, sh:32], cur[:, 0:32 - sh], ALU.add)
                cur, nxt = nxt, cur
            PEND = cur
            tt(POFF, PEND, PADA, ALU.subtract)
            tt(RANK, prk.v(prk.ap.rearrange("p (i e) -> p i e", e=32)),
               POFF.v(POFF.ap.unsqueeze(1).to_broadcast([128, NT, 32])), ALU.add)
            tmpd4 = TMPD.v(TMPD.ap.rearrange("p (i k e) -> p i k e", i=NT, k=2))
            tt(tmpd4, ohk4, RANK.v(RANK.ap.unsqueeze(2).to_broadcast([128, NT, 2, 32])), ALU.mult)
            red(DESTF, TMPD.v(TMPD.ap.rearrange("p (n e) -> p n e", e=32)), ALU.add)
            cp(DESTI, DESTF)
            tmpb3 = TMPB.v(TMPB.ap.rearrange("p (j e) -> p j e", e=32))
            tt(tmpb3, PEND.v(PEND.ap.unsqueeze(1).to_broadcast([128, NBLK, 32])),
               THR48.v(THR48.ap.unsqueeze(2).to_broadcast([128, NBLK, 32])), ALU.is_le)
            red(BEF, tmpb3, ALU.add)
            ts(BEF, BEF, 31.0, None, ALU.min)
            ts(EMP, THR48, PEND[:, 31:32], None, ALU.is_ge)
            ts(IDXF, BEF, 256.0, PIDX2, ALU.mult, ALU.add)
            ts(IDXF, IDXF, 0.5, None, ALU.mult)
            stt(IDXF, EMP, float(1 << 21), IDXF, ALU.mult, ALU.add)
            cp(IDXI, IDXF)

            IOA = bass.IndirectOffsetOnAxis
            regcache = {}

            def breg(e, v):
                if v not in regcache:
                    regcache[v] = e.to_reg(v)
                return regcache[v]


            XSB_OFF = XSB.reg[1]
            XB2 = [XB, sview(XSB_OFF + 24576, [128, 2, D], BF16)]
            XTB2 = [XTB, sview(XSB_OFF + 28672, [128, 8, 256], BF16)]

            def gather_w(j, m_):
                dT = (WG, WU, WD)[m_][j % 2]
                dflat = dT.v(dT.ap.rearrange("p k n -> p (k n)"))
                idma(lambda e, o=dflat.ap, src=wb_h[m_][:, :], idx=IDXI.ap[:, j:j + 1]:
                     e.indirect_dma_start(out=o, out_offset=None, in_=src, in_offset=IOA(ap=idx, axis=0),
                                          bounds_check=breg(e, 4095), oob_is_err=False),
                     reads=[IDXI, Tl(None, ("dram:wb%d" % m_, 0, 64))], writes=[dflat])

            def load_x(j):
                dma(XB2[j % 2], Tl(xs_h.ap()[j * 256:(j + 1) * 256, :].rearrange("(r p) f -> p r f", p=128),
                                   ("dram:xs", 0, 64)))

            def blk_B1(j):
                xb = XB2[j % 2]
                xtb = XTB2[j % 2]
                for r in range(2):
                    pt = bank()
                    ptb = bf_view(pt, 8, 128)
                    xr_ = xb.v(xb.ap[:, r, :].rearrange("p (c k) -> p k c", k=8))
                    for k in range(8):
                        tr(ptb[:, k, :], xr_[:, k, :], IDB)
                    tt(xtb[:, :, r * 128:(r + 1) * 128], ptb, bc(GFFN2, [128, 8, 128], 2), ALU.mult)

            sgi = [0]

            def blk_B2(j, hcs):
                b = j % 2
                htb = HTB[j % 2]
                xtb = XTB2[j % 2]
                for hc in hcs:
                    pg = bank()
                    pu = bank()
                    for k in range(8):
                        mm(pg[:, 0:256], WG[b][:, k, hc * 128:(hc + 1) * 128], xtb[:, k, :], k == 0, k == 7)
                    for k in range(8):
                        mm(pu[:, 0:256], WU[b][:, k, hc * 128:(hc + 1) * 128], xtb[:, k, :], k == 0, k == 7)
                    sg = SGb[sgi[0] % 2]
                    sgi[0] += 1
                    act(sg, pg[:, 0:256], AF.Silu)
                    tt(htb[:, hc, :], sg, pu[:, 0:256], ALU.mult)

            def blk_B3(j):
                b = j % 2
                htb = HTB[j % 2]
                for r in range(2):
                    for hf in range(2):
                        py_ = bank()
                        for hc in range(4):
                            mm(py_, htb[:, hc, r * 128:(r + 1) * 128], WD[b][:, hc, hf * 512:(hf + 1) * 512],
                               hc == 0, hc == 3)
                        cp(YB[:, r, hf * 512:(hf + 1) * 512], py_, eng=("act" if (r + hf) % 2 == 0 else "dve"))
                dma(Tl(ys_h.ap()[j * 256:(j + 1) * 256, :].rearrange("(r p) f -> p r f", p=128),
                       ("dram:ys", j, j + 1)), YB)

            nblk_run = NBLK
            for m_ in range(3):
                gather_w(0, m_)
            for i in range(NT):
                for k_ in range(2):
                    n_ = 2 * i + k_
                    idma(lambda e, o=xs_h[:, :], idx=DESTI.ap[:, n_:n_ + 1], src=XSB.ap[:, i, :]:
                         e.indirect_dma_start(out=o, out_offset=IOA(ap=idx, axis=0), in_=src, in_offset=None,
                                              bounds_check=breg(e, NROWS - 1), oob_is_err=False),
                         reads=[XSB, DESTI], writes=[Tl(None, ("dram:xs", n_, n_ + 1))])

            load_x(0)
            blk_B1(0)
            load_x(1)
            for j in range(nblk_run):
                nx = j + 1 < nblk_run
                if nx:
                    for m_ in range(3):
                        gather_w(j + 1, m_)

                def t_main(j=j):
                    blk_B2(j, (0, 1, 2, 3))
                    blk_B3(j)

                def t_next(j=j):
                    blk_B1(j + 1)
                    if j + 2 < nblk_run:
                        load_x(j + 2)

                th = [cap(t_main, [0, 1, 2, 3, 4, 5])]
                if nx:
                    th.append(cap(t_next, [6, 7]))
                P.merge(th)

            YGR = YG + [sview(E0 + n_ * 4096, [128, D], F32) for n_ in range(6)]
            NYG = len(YGR)

            def fin(i, th):
                R = RTS[th]
                xr = xres(i)
                for k_ in range(2):
                    n_ = 2 * i + k_
                    yg = YGR[n_ % NYG]
                    idma(lambda e, o=yg.ap, idx=DESTI.ap[:, n_:n_ + 1]:
                         e.indirect_dma_start(out=o, out_offset=None, in_=ys_h[:, :], in_offset=IOA(ap=idx, axis=0),
                                              bounds_check=breg(e, NROWS - 1), oob_is_err=False),
                         reads=[DESTI, Tl(None, ("dram:ys", 0, 64))], writes=[yg])
                    stt(xr, yg, GK[:, n_:n_ + 1], xr, ALU.mult, ALU.add)
                sq_ = YGR[(2 * i) % NYG]
                ot = OUTT[i % 2]
                act(sq_, xr, AF.Square)
                red(R.SS2, sq_, ALU.add)
                act(R.SD2, R.SS2, AF.Sqrt, scale=1.0 / D, bias=EPS_T)
                recip(R.RSTD2, R.SD2)
                stt(ot, xr, R.RSTD2, GFIN, ALU.mult, ALU.mult)
                out_dmas.append(dma(yrow(i), ot))

            for i in range(0, NT, 2):
                P.merge([cap(lambda: fin(i, 0), [0]), cap(lambda: fin(i + 1, 1), [1])])
        sink = P.add("sp", None)
        for i_, o in enumerate(P.ops):
            if o["dma"] and o["fn"] is not None:
                P.ops[sink]["deps"].add(i_)

        n_eng_sems = 4
        from contextlib import ExitStack
        with ExitStack() as es:
            eng_sems = {e: [es.enter_context(nc.semaphore(f"s_{e}{j}")) for j in range(n_eng_sems)]
                        for e in ("pe", "act", "dve", "pool")}
            dma_sems = {"main": [es.enter_context(nc.semaphore(f"s_dma{j}")) for j in range(24)],
                        "sw": [es.enter_context(nc.semaphore(f"s_sw{j}")) for j in range(16)],
                        "cv": [es.enter_context(nc.semaphore(f"s_cv{j}")) for j in range(8)]}
            block = es.enter_context(nc.Block())
            P.emit(nc, block, eng_sems, dma_sems)
    return nc, P


def _consts():
    tri = np.triu(np.ones((128, 128), np.float32))
    pa = np.zeros((4, 128, 128), np.float32)
    pb = np.zeros((4, 128, 128), np.float32)
    pa0 = np.zeros((4, 128, 128), np.float32)
    for g, w in enumerate(POOL_WINDOWS):
        for t in range(128):
            for s in range(max(t - w + 1, -128), t + 1):
                if s >= 0:
                    pa[g, s, t] += 1.0 / w
                else:
                    pb[g, s + 128, t] += 1.0 / w
            pa[g, t, t] -= 1.0
            lo = max(t - w + 1, 0)
            cnt = t + 1 - lo
            for s in range(lo, t + 1):
                pa0[g, s, t] += 1.0 / cnt
            pa0[g, t, t] -= 1.0
    cbf = np.zeros((128, NCBF), np.float32)
    cbf[:, B_ID:B_ID + 128] = np.eye(128, dtype=np.float32)
    cbf[:, B_PA:B_PA + 512] = pa.transpose(1, 0, 2).reshape(128, 512)
    cbf[:, B_PB:B_PB + 512] = pb.transpose(1, 0, 2).reshape(128, 512)
    cbf[:, B_PA0:B_PA0 + 512] = pa0.transpose(1, 0, 2).reshape(128, 512)
    cbf[:, B_TRIS:B_TRIS + 128] = np.triu(np.ones((128, 128), np.float32), 1)
    cbf[:, B_ONESB:B_ONESB + 128] = 1.0
    return tri, cbf.astype(ml_dtypes.bfloat16)


_CACHE = {}


def kernel(x, g_mix, w_in, b_if, conv_q, conv_k, g_head, w_pool, pool_scale, w_br_a, w_br_b,
           w_out, g_ffn, w_rg, b_rg, w_re, b_re, w_e_gate, w_e_up, w_e_down, g_final):
    f = np.float32
    x = np.asarray(x, f)
    tri, cbf = _consts()
    cpk = np.zeros((128, NCPK), f)
    cpk[:, C_GMIX:C_GMIX + 8] = np.asarray(g_mix, f)[0].reshape(8, 128).T
    cpk[:, C_GFFN:C_GFFN + 8] = np.asarray(g_ffn, f)[0].reshape(8, 128).T
    cpk[:, C_PSC:C_PSC + 4] = np.asarray(pool_scale, f)[0].reshape(4, 128).T
    cpk[:, C_GHEAD:C_GHEAD + 4] = np.asarray(g_head, f)[0].reshape(4, 128).T
    cpk[:, C_CQ:C_CQ + 16] = np.asarray(conv_q, f)[0].reshape(4, 4, 128).transpose(2, 1, 0).reshape(128, 16)
    cpk[:, C_CK:C_CK + 16] = np.asarray(conv_k, f)[0].reshape(4, 4, 128).transpose(2, 1, 0).reshape(128, 16)
    cpk[:, C_BIF:C_BIF + 8] = np.asarray(b_if, f)[0][None, :]
    cpk[:, C_BR:C_BR + 4] = np.asarray(b_rg, f)[0][None, :]
    cpk[:, C_BR + 4:C_BR + 36] = np.asarray(b_re, f)[0][None, :]
    cpk[:, C_EPS] = EPS
    cpk[:, C_ONE] = 1.0
    wr = np.concatenate([np.asarray(w_rg, f)[0], np.asarray(w_re, f)[0]], axis=1)
    cpk[:, C_WR:C_WR + 288] = wr.reshape(8, 128, 36).transpose(1, 0, 2).reshape(128, 288)
    cpk[:, C_IDF:C_IDF + 128] = np.eye(128, dtype=f)
    cpk[:, C_TRI:C_TRI + 128] = tri
    cpk[:, C_ONES:C_ONES + 128] = 1.0
    cpk[:, C_GFIN:C_GFIN + 1024] = np.asarray(g_final, f)[None, :]
    cpk[:, C_GFFN2:C_GFFN2 + 8] = np.asarray(g_ffn, f)[0].reshape(128, 8)
    cpk[:, C_PIDX2] = 2.0 * np.arange(128, dtype=f)
    cpk[:, C_THR16:C_THR16 + 16] = 256.0 * np.arange(16, dtype=f)[None, :]
    cpk[:, C_THR48:C_THR48 + 48] = 256.0 * np.arange(48, dtype=f)[None, :]

    key = (STAGE, N_EXPERTS_RUN)
    if key not in _CACHE:
        _CACHE[key] = build_nc()[0]
    nc = _CACHE[key]
    shared = {
        "w_in": np.ascontiguousarray(np.asarray(w_in, f)[0]),
        "w_pool": np.ascontiguousarray(np.asarray(w_pool, f)[0]),
        "w_br_a": np.ascontiguousarray(np.asarray(w_br_a, f)[0]),
        "w_br_b": np.ascontiguousarray(np.asarray(w_br_b, f)[0]),
        "w_out": np.ascontiguousarray(np.asarray(w_out, f)[0]),
        "w_e_gate": np.ascontiguousarray(np.asarray(w_e_gate, f)[0]).reshape(8192, 2048),
        "w_e_up": np.ascontiguousarray(np.asarray(w_e_up, f)[0]).reshape(8192, 2048),
        "w_e_down": np.ascontiguousarray(np.asarray(w_e_down, f)[0]).reshape(8192, 2048),
        "zx": np.zeros((128, 16384), ml_dtypes.bfloat16),
        "cpk": cpk,
        "cbf": cbf,
    }
    in_maps = []
    for c in range(8):
        m = dict(shared)
        m["x"] = np.ascontiguousarray(x[c])
        in_maps.append(m)
    res = run_bass_kernel_spmd(nc, in_maps, core_ids=list(range(8)))
    return np.stack([np.asarray(r["y"], f) for r in res.results], axis=0)
```
